# Optimizing a Trainium2 kernel written in Bass

```python
import jax, jax.numpy as jnp
from jax import lax
import numpy as np

D_MODEL = 1024
BATCH = 1
SEQ = 16384
DEPTH = 2

CHUNK = 64
N_MEM = 256
ROPE_THETA = 10000.0
LN_EPS = 1e-5

POOL_GROUPS = 4
POOL_GROUP_DIM = 128
POOL_WINDOWS = (2, 4, 8, 16)
POOL_WIDTH = POOL_GROUPS * POOL_GROUP_DIM
ATT_HEADS = 8
ATT_HEAD_DIM = 64
ATT_WIDTH = ATT_HEADS * ATT_HEAD_DIM
IDX_HEADS = 8
IDX_HEAD_DIM = 64
TOPK_MAX = 256
Q_BLOCK = 128
MEM_HEADS = 4
MEM_HEAD_DIM = 128
MEM_WIDTH = MEM_HEADS * MEM_HEAD_DIM
N_BRANCH = 3
BRANCH_WIDTH = 512
IN_WIDTHS = (POOL_WIDTH, ATT_WIDTH, ATT_WIDTH, ATT_WIDTH, IDX_HEADS * IDX_HEAD_DIM,
             IDX_HEAD_DIM, IDX_HEADS, MEM_WIDTH, N_BRANCH * D_MODEL)
D_IN = sum(IN_WIDTHS)
N_EXPERTS = 16
N_GROUPS = 4
EXPERTS_PER_GROUP = N_EXPERTS // N_GROUPS
TOP_K_EXPERTS = 2
GROUP_SCORE_TOPK = 2
D_EXPERT = 512
MOE_BLOCK = 128
DN_ALPHA = (2 * DEPTH) ** 0.25
DN_BETA = (8 * DEPTH) ** -0.25

kernel_name = 'hybrid_pool_dsa_mem_moe_deepnorm'


def layer_norm(x, g, b):
    xf = x.astype(jnp.float32)
    mu = jnp.mean(xf, axis=-1, keepdims=True)
    var = jnp.mean(jnp.square(xf - mu), axis=-1, keepdims=True)
    y = (xf - mu) * lax.rsqrt(var + LN_EPS) * g.astype(jnp.float32) + b.astype(jnp.float32)
    return y.astype(x.dtype)


def rotary(x, positions):
    half = x.shape[-1] // 2
    inv_freq = ROPE_THETA ** (-jnp.arange(half, dtype=jnp.float32) / half)
    ang = positions.astype(jnp.float32)[..., None] * inv_freq
    cos = jnp.cos(ang)[:, :, None, :]
    sin = jnp.sin(ang)[:, :, None, :]
    xf = x.astype(jnp.float32)
    x1, x2 = xf[..., :half], xf[..., half:]
    out = jnp.concatenate([x1 * cos - x2 * sin, x2 * cos + x1 * sin], axis=-1)
    return out.astype(x.dtype)


def pool_branch(u, pool_w, pool_scale):
    B, S, _ = u.shape
    u = u.reshape(B, S, POOL_GROUPS, POOL_GROUP_DIM)
    t = jnp.arange(S)
    outs = []
    for g, w in enumerate(POOL_WINDOWS):
        ug = u[:, :, g].astype(jnp.float32)
        cs = jnp.cumsum(ug, axis=1)
        lagged = jnp.pad(cs, ((0, 0), (w, 0), (0, 0)))[:, :S]
        cnt = jnp.minimum(t + 1, w).astype(jnp.float32)[None, :, None]
        outs.append((cs - lagged) / cnt - ug)
    d = jnp.stack(outs, axis=2).astype(u.dtype)
    mixed = jnp.einsum('bsgc,gce->bsge', d, pool_w).reshape(B, S, POOL_WIDTH)
    return mixed * pool_scale


def dsa_branch(q, k, v, iq, ik, iw, positions):
    B, S, _ = q.shape
    q = rotary(q.reshape(B, S, ATT_HEADS, ATT_HEAD_DIM), positions)
    k = rotary(k.reshape(B, S, ATT_HEADS, ATT_HEAD_DIM), positions)
    v = v.reshape(B, S, ATT_HEADS, ATT_HEAD_DIM)
    iq = rotary(iq.reshape(B, S, IDX_HEADS, IDX_HEAD_DIM), positions)
    ik = rotary(ik[:, :, None, :], positions)[:, :, 0]
    iw = iw * (IDX_HEADS ** -0.5)
    n_sel = min(TOPK_MAX, S // 4)
    nb = S // Q_BLOCK
    key_chunk = jnp.arange(S) // CHUNK

    def to_blocks(a):
        return a.reshape(B, nb, Q_BLOCK, *a.shape[2:]).swapaxes(0, 1)

    def attend_block(args):
        blk, qb, iqb, iwb = args
        t = blk * Q_BLOCK + jnp.arange(Q_BLOCK)
        q_chunk = t // CHUNK
        admissible = key_chunk[None, :] <= q_chunk[:, None]
        rel = jax.nn.relu(jnp.einsum('bqhd,bsd->bqhs', iqb, ik) * (IDX_HEAD_DIM ** -0.5))
        score = jnp.einsum('bqhs,bqh->bqs', rel, iwb).astype(jnp.float32)
        score = jnp.where(admissible[None], score, -jnp.inf)
        _, sel = lax.top_k(score, n_sel)
        valid = (sel // CHUNK) <= q_chunk[None, :, None]
        kg = jax.vmap(lambda kk, ii: kk[ii])(k, sel)
        vg = jax.vmap(lambda vv, ii: vv[ii])(v, sel)
        s = jnp.einsum('bqhd,bqkhd->bqhk', qb, kg).astype(jnp.float32) * (ATT_HEAD_DIM ** -0.5)
        s = jnp.where(valid[:, :, None, :], s, -jnp.inf)
        p = jax.nn.softmax(s, axis=-1).astype(vg.dtype)
        return jnp.einsum('bqhk,bqkhd->bqhd', p, vg)

    out = lax.map(attend_block, (jnp.arange(nb), to_blocks(q), to_blocks(iq), to_blocks(iw)))
    return out.swapaxes(0, 1).reshape(B, S, ATT_WIDTH)


def memory_branch(mq, mem, w_mem_kv):
    B, S, _ = mq.shape
    M = mem.shape[1]
    kv = jnp.einsum('bmd,de->bme', mem, w_mem_kv)
    mk, mv = jnp.split(kv, 2, axis=-1)
    mk = mk.reshape(B, M, MEM_HEADS, MEM_HEAD_DIM)
    mv = mv.reshape(B, M, MEM_HEADS, MEM_HEAD_DIM)
    q = mq.reshape(B, S, MEM_HEADS, MEM_HEAD_DIM)
    s = jnp.einsum('bshd,bmhd->bhsm', q, mk).astype(jnp.float32) * (MEM_HEAD_DIM ** -0.5)
    p = jax.nn.softmax(s, axis=-1).astype(mv.dtype)
    return jnp.einsum('bhsm,bmhd->bshd', p, mv).reshape(B, S, MEM_WIDTH)


def token_mixing(x, mem, positions, w_in, b_in, pool_w, pool_scale, w_mem_kv, w_br, w_out):
    B, S, D = x.shape
    proj = jnp.einsum('bsd,de->bse', x, w_in) + b_in
    split_points = np.cumsum(IN_WIDTHS)[:-1].tolist()
    u_pool, q, k, v, iq, ik, iw, mq, gate_logits = jnp.split(proj, split_points, axis=-1)
    gates = jax.nn.sigmoid(gate_logits.astype(jnp.float32)).astype(x.dtype).reshape(B, S, N_BRANCH, D)
    branches = (pool_branch(u_pool, pool_w, pool_scale),
                dsa_branch(q, k, v, iq, ik, iw, positions),
                memory_branch(mq, mem, w_mem_kv))
    merged = jnp.zeros_like(x)
    for n in range(N_BRANCH):
        merged = merged + gates[:, :, n] * jnp.einsum('bsc,cd->bsd', branches[n], w_br[n])
    return jnp.einsum('bsd,de->bse', merged, w_out)


def route(x2, w_router, router_bias):
    scores = jax.nn.sigmoid(jnp.einsum('nd,de->ne', x2, w_router).astype(jnp.float32))
    biased = scores + router_bias.astype(jnp.float32)
    grouped = biased.reshape(-1, N_GROUPS, EXPERTS_PER_GROUP)
    group_score = lax.top_k(grouped, GROUP_SCORE_TOPK)[0].sum(-1)
    best = jnp.argmax(group_score, axis=-1).astype(jnp.int32)
    within = jnp.take_along_axis(grouped, best[:, None, None], axis=1)[:, 0]
    _, local = lax.top_k(within, TOP_K_EXPERTS)
    expert_idx = (best[:, None] * EXPERTS_PER_GROUP + local).astype(jnp.int32)
    w = jnp.take_along_axis(scores, expert_idx, axis=1)
    w = w / jnp.sum(w, axis=-1, keepdims=True)
    return expert_idx, w


def moe(x2, w_router, router_bias, w1, w3, w2):
    N, D = x2.shape
    expert_idx, gate_w = route(x2, w_router, router_bias)
    n_assign = N * TOP_K_EXPERTS
    n_blk = -(-n_assign // MOE_BLOCK) + N_EXPERTS
    flat_e = expert_idx.reshape(-1)
    flat_tok = jnp.repeat(jnp.arange(N, dtype=jnp.int32), TOP_K_EXPERTS)
    flat_g = gate_w.reshape(-1).astype(x2.dtype)
    order = jnp.argsort(flat_e)
    e_sorted = flat_e[order]
    counts = jax.ops.segment_sum(jnp.ones_like(flat_e), flat_e, num_segments=N_EXPERTS)
    padded = ((counts + MOE_BLOCK - 1) // MOE_BLOCK) * MOE_BLOCK
    pad_end = jnp.cumsum(padded)
    pad_start = pad_end - padded
    start = jnp.cumsum(counts) - counts
    rank = jnp.arange(n_assign, dtype=jnp.int32) - start[e_sorted]
    dest = pad_start[e_sorted] + rank
    slot_tok = jnp.full((n_blk * MOE_BLOCK,), N, jnp.int32).at[dest].set(flat_tok[order])
    slot_gate = jnp.zeros((n_blk * MOE_BLOCK,), x2.dtype).at[dest].set(flat_g[order])
    blk_e = jnp.minimum(jnp.searchsorted(pad_end, jnp.arange(n_blk) * MOE_BLOCK, side='right'),
                        N_EXPERTS - 1).astype(jnp.int32)
    x_pad = jnp.concatenate([x2, jnp.zeros((1, D), x2.dtype)], axis=0)

    def run_block(args):
        e, toks = args
        xb = x_pad[toks]
        h = jax.nn.silu(xb @ w1[e]) * (xb @ w3[e])
        return h @ w2[e]

    yb = lax.map(run_block, (blk_e, slot_tok.reshape(n_blk, MOE_BLOCK)))
    y = yb.reshape(-1, D) * slot_gate[:, None]
    return jnp.zeros((N + 1, D), x2.dtype).at[slot_tok].add(y)[:N]


def setup_inputs(seed: int = 0) -> dict:
    key = jax.random.key(seed)
    ks = jax.random.split(key, 20)
    f32 = jnp.float32
    nrm = lambda k, shape, scale: jax.random.normal(k, shape, f32) * scale
    return {
        'x': nrm(ks[0], (BATCH, SEQ, D_MODEL), 1.0),
        'mem': nrm(ks[1], (BATCH, N_MEM, D_MODEL), 1.0),
        'positions': jnp.broadcast_to(jnp.arange(SEQ, dtype=jnp.int32)[None], (BATCH, SEQ)),
        'w_in': nrm(ks[2], (DEPTH, D_MODEL, D_IN), D_MODEL ** -0.5),
        'b_in': nrm(ks[3], (DEPTH, D_IN), 0.02),
        'pool_w': nrm(ks[4], (DEPTH, POOL_GROUPS, POOL_GROUP_DIM, POOL_GROUP_DIM), POOL_GROUP_DIM ** -0.5),
        'pool_scale': 1.0 + nrm(ks[5], (DEPTH, POOL_WIDTH), 0.02),
        'w_mem_kv': nrm(ks[6], (DEPTH, D_MODEL, 2 * MEM_WIDTH), D_MODEL ** -0.5),
        'w_br': nrm(ks[7], (DEPTH, N_BRANCH, BRANCH_WIDTH, D_MODEL), DN_BETA * BRANCH_WIDTH ** -0.5),
        'w_out': nrm(ks[8], (DEPTH, D_MODEL, D_MODEL), DN_BETA * D_MODEL ** -0.5),
        'ln1_g': 1.0 + nrm(ks[9], (DEPTH, D_MODEL), 0.02),
        'ln1_b': nrm(ks[10], (DEPTH, D_MODEL), 0.02),
        'w_router': nrm(ks[11], (D_MODEL, N_EXPERTS), D_MODEL ** -0.5),
        'router_bias': nrm(ks[12], (N_EXPERTS,), 0.01),
        'w1': nrm(ks[13], (DEPTH, N_EXPERTS, D_MODEL, D_EXPERT), D_MODEL ** -0.5),
        'w3': nrm(ks[14], (DEPTH, N_EXPERTS, D_MODEL, D_EXPERT), D_MODEL ** -0.5),
        'w2': nrm(ks[15], (DEPTH, N_EXPERTS, D_EXPERT, D_MODEL), DN_BETA * D_EXPERT ** -0.5),
        'ln2_g': 1.0 + nrm(ks[16], (DEPTH, D_MODEL), 0.02),
        'ln2_b': nrm(ks[17], (DEPTH, D_MODEL), 0.02),
    }


def reference(x, mem, positions, w_in, b_in, pool_w, pool_scale, w_mem_kv, w_br, w_out,
              ln1_g, ln1_b, w_router, router_bias, w1, w3, w2, ln2_g, ln2_b):
    B, S, D = x.shape
    for l in range(DEPTH):
        mix = token_mixing(x, mem, positions, w_in[l], b_in[l], pool_w[l], pool_scale[l],
                           w_mem_kv[l], w_br[l], w_out[l])
        x = layer_norm(DN_ALPHA * x + mix, ln1_g[l], ln1_b[l])
        ffn = moe(x.reshape(B * S, D), w_router, router_bias, w1[l], w3[l], w2[l]).reshape(B, S, D)
        x = layer_norm(DN_ALPHA * x + ffn, ln2_g[l], ln2_b[l])
    return x
```

```python
import numpy as np
import ml_dtypes
import concourse.bass as bass
import concourse.mybir as mybir
from concourse.bass_utils import run_bass_kernel_spmd

F32 = mybir.dt.float32
BF16 = mybir.dt.bfloat16
I32 = mybir.dt.int32
U8 = mybir.dt.uint8
AF = mybir.ActivationFunctionType
ALU = mybir.AluOpType
AX = mybir.AxisListType

NCORES = 8
S = 16384
D = 1024
TL = 2048
NTL = 16
DIN = 6216
ALPHA = 4.0 ** 0.25
LN_EPS = 1e-5
NIT = 26
BIS_W = 4096.0
OFF = dict(pool=0, q=512, k=1024, v=1536, iq=2048, ik=2560, iw=2624, mq=2632, gate=3144)
ROFF = dict(q=0, k=512, iq=1024, ik=1536)


class Sem:
    def __init__(self, handle, step):
        self.h = handle
        self.step = step
        self.count = 0


class Tile:
    __slots__ = ("name", "w", "r")

    def __init__(self, name=""):
        self.name = name
        self.w = None
        self.r = {}


class Q:
    def __init__(self, name, sem):
        self.name = name
        self.sem = sem
        self.waited = {}
        self.ops = []
        self.pending = False


class Sync:
    def __init__(self, nc, n_dma_sems=48):
        self.nc = nc
        self.stack = []
        self.qs = {}
        for name in ("pe", "dve", "act", "pool", "sp"):
            cm = nc.semaphore("s_" + name)
            h = cm.__enter__()
            self.stack.append(cm)
            self.qs[name] = Q(name, Sem(h, 1))
        self.dma_sems = []
        for i in range(n_dma_sems):
            cm = nc.semaphore("d%d" % i)
            h = cm.__enter__()
            self.stack.append(cm)
            self.dma_sems.append(Sem(h, 16))
        self.dma_rr = 0

    def close(self):
        for cm in reversed(self.stack):
            cm.__exit__(None, None, None)

    def _needs(self, reads, writes):
        need = {}

        def add(s, v):
            if need.get(s, 0) < v:
                need[s] = v
        for t in reads:
            if t.w is not None:
                add(*t.w)
        for t in writes:
            if t.w is not None:
                add(*t.w)
            for s, v in t.r.items():
                add(s, v)
        return need

    def _emit_waits(self, q, need, skip_self=False):
        for s, v in need.items():
            if skip_self and s is q.sem:
                continue
            if q.waited.get(s, 0) >= v:
                continue
            q.waited[s] = v
            q.ops.append(("wait", s, v))

    def op(self, qname, fn, reads=(), writes=(), signal=True):
        q = self.qs[qname]
        need = self._needs(reads, writes)
        is_pe = qname == "pe"
        self._emit_waits(q, need, skip_self=is_pe)
        if not is_pe:
            signal = True
        if signal:
            q.sem.count += 1
            val = q.sem.count
            q.ops.append(("ins", fn, q.sem, 1))
            q.pending = False
        else:
            val = q.sem.count + 1
            q.ops.append(("ins", fn, None, 0))
            q.pending = True
        for t in reads:
            if t.r.get(q.sem, 0) < val:
                t.r[q.sem] = val
        for t in writes:
            t.w = (q.sem, val)
            t.r = {}
        return val

    def dma(self, qname, fn, reads=(), writes=()):
        q = self.qs[qname]
        need = self._needs(reads, writes)
        sem = self.dma_sems[self.dma_rr % len(self.dma_sems)]
        self.dma_rr += 1
        if sem.count > 0:
            need[sem] = max(need.get(sem, 0), sem.count)
        self._emit_waits(q, need)
        sem.count += 16
        val = sem.count
        q.ops.append(("ins", fn, sem, 16))
        for t in reads:
            if t.r.get(sem, 0) < val:
                t.r[sem] = val
        for t in writes:
            t.w = (sem, val)
            t.r = {}

    def barrier(self):
        allsems = [q.sem for q in self.qs.values()] + self.dma_sems
        for q in self.qs.values():
            assert not q.pending
            need = {s: s.count for s in allsems if s.count > 0 and s is not q.sem}
            self._emit_waits(q, need)

    def flush(self):
        nc = self.nc
        for q in self.qs.values():
            assert not q.pending, "PE has un-signalled trailing instructions"
        with nc.Block() as block:
            def mk(q):
                def body(eng):
                    for o in q.ops:
                        if o[0] == "wait":
                            eng.wait_ge(o[1].h, o[2])
                        else:
                            ins = o[1](eng)
                            if o[2] is not None:
                                ins.then_inc(o[2].h, o[3])
                return body
            block.tensor(mk(self.qs["pe"]))
            block.vector(mk(self.qs["dve"]))
            block.scalar(mk(self.qs["act"]))
            block.gpsimd(mk(self.qs["pool"]))
            block.sync(mk(self.qs["sp"]))
        for q in self.qs.values():
            q.ops = []


class Prog:
    def __init__(self, ext_in, ext_out, internal):
        self.nc = bass.Bass("TRN2", target_bir_lowering=False)
        nc = self.nc
        self.d = {}
        self.dt = {}
        for name, (shape, dt) in ext_in.items():
            self.d[name] = nc.dram_tensor(name, list(shape), dt, kind="ExternalInput").ap()
            self.dt[name] = Tile(name)
        for name, (shape, dt) in ext_out.items():
            self.d[name] = nc.dram_tensor(name, list(shape), dt, kind="ExternalOutput").ap()
            self.dt[name] = Tile(name)
        for name, (shape, dt) in internal.items():
            self.d[name] = nc.dram_tensor(name, list(shape), dt, kind="Internal").ap()
            self.dt[name] = Tile(name)
        self.sy = Sync(nc)
        self.ctxs = []
        self.out_names = list(ext_out.keys())

    def sb(self, name, shape, dt):
        cm = self.nc.sbuf_tensor(name, list(shape), dt)
        h = cm.__enter__()
        self.ctxs.append(cm)
        return h

    def ps(self, name, shape, dt):
        cm = self.nc.psum_tensor(name, list(shape), dt)
        h = cm.__enter__()
        self.ctxs.append(cm)
        return h

    def mark(self):
        return len(self.ctxs)

    def release(self, mark):
        self.sy.barrier()
        self.sy.flush()
        while len(self.ctxs) > mark:
            self.ctxs.pop().__exit__(None, None, None)

    def finish(self):
        sy = self.sy
        q = sy.qs["sp"]
        need = {}
        for n in self.out_names:
            t = self.dt[n]
            if t.w is not None:
                s, v = t.w
                need[s] = max(need.get(s, 0), v)
        sy._emit_waits(q, need)
        sy.barrier()
        sy.flush()
        while self.ctxs:
            self.ctxs.pop().__exit__(None, None, None)
        sy.close()


def bcast_rows(ap, nparts):
    return ap.partition_broadcast(nparts)


def load_consts(P, C):
    nc, sy, d, dt = P.nc, P.sy, P.d, P.dt
    C["ident"] = P.sb("ident", [128, 128], BF16)
    C["identf"] = P.sb("identf", [128, 128], F32)
    C["ones"] = P.sb("ones", [128, 128], BF16)
    C["t_const"] = Tile("const")
    tc_ = C["t_const"]
    sy.dma("pool", lambda e: e.dma_start(out=C["ident"][:], in_=d["c_ident"]), reads=[dt["c_ident"]], writes=[tc_])
    sy.dma("sp", lambda e: e.dma_start(out=C["identf"][:], in_=d["c_ident"]), reads=[dt["c_ident"]], writes=[tc_])
    sy.op("dve", lambda e: e.memset(C["ones"][:], 1.0), writes=[tc_])
    C["bank"] = [P.ps("bank%d" % i, [128, 512], F32) for i in range(7)]
    C["bankb"] = P.ps("bankb", [128, 1024], BF16)
    C["tb"] = [Tile("bank%d" % i) for i in range(7)]
    C["tbb"] = Tile("bankb")


def alloc_rope(P, C):
    C["cosT"] = P.sb("cosT", [128, TL], BF16)
    C["sinT"] = P.sb("sinT", [128, TL], BF16)
    C["t_rope"] = Tile("rope")


def phase_X(P, C, xname):
    nc, sy, d, dt = P.nc, P.sy, P.d, P.dt
    xT = C["xT"]
    t_xT = C["t_xT"]
    m = P.mark()
    xb = [P.sb("xb%d" % i, [128, 1024], BF16) for i in range(2)]
    t_xb = [Tile("xb%d" % i) for i in range(2)]
    for j in range(NTL):
        b = j % 2
        sy.dma("pool", lambda e, j=j, b=b: e.dma_start(out=xb[b][:], in_=d[xname][j * 128:(j + 1) * 128, :]),
               reads=[dt[xname]], writes=[t_xb[b]])
        for k in range(8):
            sy.op("pe", lambda e, k=k, b=b: e.transpose(C["bankb"][:, k * 128:(k + 1) * 128], xb[b][:, k * 128:(k + 1) * 128], C["ident"][:]),
                  reads=[t_xb[b], C["t_const"]], writes=[C["tbb"]], signal=(k == 7))
        sy.op("dve", lambda e, j=j: e.tensor_copy(xT[:, :, j * 128:(j + 1) * 128],
                                                   C["bankb"][:].rearrange("p (k t) -> p k t", k=8)),
              reads=[C["tbb"]], writes=[t_xT])
    cosT, sinT, t_rope = C["cosT"], C["sinT"], C["t_rope"]
    posi = P.sb("posi", [128, TL], I32)
    y = P.sb("ropy", [128, TL], F32)
    f = P.sb("ropf", [128, TL], F32)
    ki = P.sb("ropk", [128, TL], I32)
    kf = P.sb("ropkf", [128, TL], F32)
    t1 = P.sb("ropt1", [128, TL], F32)
    rc = P.sb("ropc", [128, 4], F32)
    tt = Tile("ropetmp")
    sy.dma("sp", lambda e: e.dma_start(out=posi[:], in_=bcast_rows(d["pos"], 128)), reads=[dt["pos"]], writes=[tt])
    sy.dma("sp", lambda e: e.dma_start(out=rc[:], in_=d["c_rope"]), reads=[dt["c_rope"]], writes=[tt])
    sy.op("dve", lambda e: e.tensor_copy(y[:], posi[:]), reads=[tt], writes=[tt])
    sy.op("dve", lambda e: e.tensor_scalar(y[:], y[:], rc[:, 0:1], float(1.0 / (2 * np.pi)), ALU.mult, ALU.mult), reads=[tt], writes=[tt])
    for which in (0, 1):
        if which == 1:
            sy.op("dve", lambda e: e.tensor_scalar(y[:], y[:], 0.25, None, ALU.add), reads=[tt], writes=[tt])
        sy.op("dve", lambda e: e.tensor_copy(ki[:], y[:]), reads=[tt], writes=[tt])
        sy.op("dve", lambda e: e.tensor_copy(kf[:], ki[:]), reads=[tt], writes=[tt])
        sy.op("dve", lambda e: e.tensor_tensor(f[:], y[:], kf[:], ALU.subtract), reads=[tt], writes=[tt])
        sy.op("dve", lambda e: e.tensor_scalar(t1[:], f[:], 0.5, None, ALU.is_gt), reads=[tt], writes=[tt])
        sy.op("dve", lambda e: e.tensor_tensor(f[:], f[:], t1[:], ALU.subtract), reads=[tt], writes=[tt])
        sy.op("dve", lambda e: e.tensor_scalar(t1[:], f[:], -0.5, None, ALU.is_lt), reads=[tt], writes=[tt])
        sy.op("dve", lambda e: e.tensor_tensor(f[:], f[:], t1[:], ALU.add), reads=[tt], writes=[tt])
        if which == 0:
            sy.op("act", lambda e: e.activation(sinT[:], f[:], AF.Sin, scale=rc[:, 1:2]), reads=[tt], writes=[t_rope])
        else:
            sy.op("act", lambda e: e.activation(cosT[:], f[:], AF.Sin, scale=rc[:, 2:3]), reads=[tt], writes=[t_rope])
    P.release(m)


def load_w(P, name, dst, cols, ncols, q="pool"):
    src = P.d[name][:, cols:cols + ncols].rearrange("(k p) c -> p k c", p=128)
    return lambda e: e.dma_start(out=dst, in_=src)


def fproj(P, C, wname, wcol, rname, rcol, ncols, bcol, brcol, out_name, W, Wr, tW, tmp):
    nc, sy, d, dt = P.nc, P.sy, P.d, P.dt
    xT, t_xT = C["xT"], C["t_xT"]
    bank, tb = C["bank"], C["tb"]
    bc = C["bcols"]
    rot = rname is not None
    sy.dma("pool", load_w(P, wname, W[:, :, 0:ncols], wcol, ncols), reads=[dt[wname]], writes=[tW[0]])
    if rot:
        sy.dma("pool", load_w(P, rname, Wr[:, :, 0:ncols], rcol, ncols), reads=[dt[rname]], writes=[tW[1]])
    nchunk = (ncols + 127) // 128
    outT, t_out, o1, o2, t_o = tmp
    for c in range(nchunk):
        m = min(128, ncols - c * 128)
        for tg in range(4):
            pa, pb = bank[(2 * tg) % 4], bank[(2 * tg + 1) % 4]
            ta, tbb_ = tb[(2 * tg) % 4], tb[(2 * tg + 1) % 4]
            for k in range(8):
                sy.op("pe", lambda e, c=c, m=m, k=k, tg=tg, pa=pa: e.matmul(pa[0:m, :], W[:, k, c * 128:c * 128 + m], xT[:, k, tg * 512:(tg + 1) * 512], start=(k == 0), stop=(k == 7)),
                      reads=[tW[0], t_xT], writes=[ta], signal=(k == 7))
            if rot:
                for k in range(8):
                    sy.op("pe", lambda e, c=c, m=m, k=k, tg=tg, pb=pb: e.matmul(pb[0:m, :], Wr[:, k, c * 128:c * 128 + m], xT[:, k, tg * 512:(tg + 1) * 512], start=(k == 0), stop=(k == 7)),
                          reads=[tW[1], t_xT], writes=[tbb_], signal=(k == 7))
                i = tg % 2
                sy.op("dve", lambda e, c=c, m=m, tg=tg, pa=pa, i=i: e.scalar_tensor_tensor(o1[i][0:m, :], pa[0:m, :], bc[0:m, bcol + c:bcol + c + 1], C["cosT"][0:m, tg * 512:(tg + 1) * 512], ALU.add, ALU.mult),
                      reads=[ta, C["t_rope"], C["t_const"]], writes=[t_o[i]])
                sy.op("dve", lambda e, c=c, m=m, tg=tg, pb=pb, i=i: e.scalar_tensor_tensor(o2[i][0:m, :], pb[0:m, :], bc[0:m, brcol + c:brcol + c + 1], C["sinT"][0:m, tg * 512:(tg + 1) * 512], ALU.add, ALU.mult),
                      reads=[tbb_, C["t_rope"], C["t_const"]], writes=[t_o[2 + i]])
                sy.op("pool", lambda e, c=c, m=m, tg=tg, i=i: e.tensor_tensor(outT[0:m, c, tg * 512:(tg + 1) * 512], o1[i][0:m, :], o2[i][0:m, :], ALU.add),
                      reads=[t_o[i], t_o[2 + i]], writes=[t_out])
            else:
                sy.op("act", lambda e, c=c, m=m, tg=tg, pa=pa: e.activation(outT[0:m, c, tg * 512:(tg + 1) * 512], pa[0:m, :], AF.Identity, bias=bc[0:m, bcol + c:bcol + c + 1]),
                      reads=[ta, C["t_const"]], writes=[t_out])
    if ncols >= 128:
        dst = d[out_name].rearrange("(c p) t -> p c t", p=128)
        sy.dma("sp", lambda e: e.dma_start(out=dst, in_=outT[:, 0:nchunk, :]), reads=[t_out], writes=[dt[out_name]])
    else:
        sy.dma("sp", lambda e: e.dma_start(out=d[out_name], in_=outT[0:ncols, 0, :]), reads=[t_out], writes=[dt[out_name]])


def tproj(P, C, wname, wcol, ncols, brow_name, W, tW, out_name, out_dt, osb, t_osb, rows=None, brow=None, t_brow=None):
    nc, sy, d, dt = P.nc, P.sy, P.d, P.dt
    xT, t_xT = C["xT"], C["t_xT"]
    bank, tb = C["bank"], C["tb"]
    sy.dma("pool", load_w(P, wname, W[:, :, 0:ncols], wcol, ncols), reads=[dt[wname]], writes=[tW])
    sy.dma("pool", lambda e: e.dma_start(out=brow[0:1, 0:ncols], in_=d[brow_name][wcol:wcol + ncols].rearrange("(o n) -> o n", o=1)),
           reads=[dt[brow_name]], writes=[t_brow])
    for j in range(NTL):
        pa, ta = bank[4 + j % 2], tb[4 + j % 2]
        if rows is None:
            t0, mrows = j * 128, 128
        else:
            t0, mrows = j * 128 + 128 - rows, rows
        for k in range(8):
            sy.op("pe", lambda e, k=k, pa=pa, t0=t0, mrows=mrows: e.matmul(pa[0:mrows, 0:ncols], xT[:, k, t0:t0 + mrows], W[:, k, 0:ncols], start=(k == 0), stop=False),
                  reads=[tW, t_xT], writes=[ta], signal=False)
        sy.op("pe", lambda e, pa=pa, mrows=mrows: e.matmul(pa[0:mrows, 0:ncols], C["ones"][0:1, 0:mrows], brow[0:1, 0:ncols], start=False, stop=True),
              reads=[t_brow, C["t_const"]], writes=[ta], signal=True)
        i = j % 2
        sy.op("act", lambda e, pa=pa, mrows=mrows, i=i: e.activation(osb[i][0:mrows, 0:ncols], pa[0:mrows, 0:ncols], AF.Copy),
              reads=[ta], writes=[t_osb[i]])
        if rows is None:
            sy.dma("sp", lambda e, j=j, i=i: e.dma_start(out=d[out_name][j * 128:(j + 1) * 128, :], in_=osb[i][:, 0:ncols]),
                   reads=[t_osb[i]], writes=[dt[out_name]])
        else:
            sy.dma("sp", lambda e, j=j, i=i: e.dma_start(out=d[out_name][j * rows:(j + 1) * rows, :], in_=osb[i][0:rows, 0:ncols]),
                   reads=[t_osb[i]], writes=[dt[out_name]])


def phase_A(P, C, L):
    nc, sy, d, dt = P.nc, P.sy, P.d, P.dt
    m = P.mark()
    W = P.sb("A_W", [128, 8, 512], BF16)
    Wr = P.sb("A_Wr", [128, 8, 512], BF16)
    tW = [Tile("A_W"), Tile("A_Wr")]
    outT = P.sb("A_outT", [128, 4, TL], BF16)
    o1 = [P.sb("A_o1%d" % i, [128, 512], F32) for i in range(2)]
    o2 = [P.sb("A_o2%d" % i, [128, 512], F32) for i in range(2)]
    tmp = (outT, Tile("A_outT"), o1, o2, [Tile("A_o%d" % i) for i in range(4)])
    fproj(P, C, "w_in" + L, OFF["k"], "w_rot" + L, ROFF["k"], 512, C["bc_k"], C["bc_kr"], "pay_k", W, Wr, tW, tmp)
    fproj(P, C, "w_in" + L, OFF["ik"], "w_rot" + L, ROFF["ik"], 64, C["bc_ik"], C["bc_ikr"], "pay_ik", W, Wr, tW, tmp)
    osb = [P.sb("A_osb%d" % i, [128, 512], BF16) for i in range(2)]
    t_osb = [Tile("A_osb%d" % i) for i in range(2)]
    brow = P.sb("A_brow", [1, 512], BF16)
    t_brow = Tile("A_brow")
    tproj(P, C, "w_in" + L, OFF["v"], 512, "b_in" + L, W, tW[0], "pay_v", BF16, osb, t_osb, brow=brow, t_brow=t_brow)
    tproj(P, C, "w_in" + L, OFF["pool"], 512, "b_in" + L, W, tW[0], "pay_ut", BF16, osb, t_osb, rows=16, brow=brow, t_brow=t_brow)
    P.release(m)


def phase_B1(P, C, L):
    nc, sy, d, dt = P.nc, P.sy, P.d, P.dt
    m = P.mark()
    W = P.sb("B1_W", [128, 8, 512], BF16)
    Wr = P.sb("B1_Wr", [128, 8, 512], BF16)
    tW = [Tile("B1_W"), Tile("B1_Wr")]
    outT = P.sb("B1_outT", [128, 4, TL], BF16)
    o1 = [P.sb("B1_o1%d" % i, [128, 512], F32) for i in range(2)]
    o2 = [P.sb("B1_o2%d" % i, [128, 512], F32) for i in range(2)]
    tmp = (outT, Tile("B1_outT"), o1, o2, [Tile("B1_o%d" % i) for i in range(4)])
    fproj(P, C, "w_in" + L, OFF["q"], "w_rot" + L, ROFF["q"], 512, C["bc_q"], C["bc_qr"], "qT_s", W, Wr, tW, tmp)
    fproj(P, C, "w_in" + L, OFF["iq"], "w_rot" + L, ROFF["iq"], 512, C["bc_iq"], C["bc_iqr"], "iqT_s", W, Wr, tW, tmp)
    fproj(P, C, "w_in" + L, OFF["mq"], None, 0, 512, C["bc_mq"], 0, "mqT_s", W, Wr, tW, tmp)
    osb = [P.sb("B1_osb%d" % i, [128, 512], BF16) for i in range(2)]
    osf = [P.sb("B1_osf%d" % i, [128, 8], F32) for i in range(2)]
    t_osb = [Tile("B1_osb%d" % i) for i in range(2)]
    t_osf = [Tile("B1_osf%d" % i) for i in range(2)]
    brow = P.sb("B1_brow", [1, 512], BF16)
    t_brow = Tile("B1_brow")
    tproj(P, C, "w_in" + L, OFF["pool"], 512, "b_in" + L, W, tW[0], "up_s", BF16, osb, t_osb, brow=brow, t_brow=t_brow)
    tproj(P, C, "w_in" + L, OFF["iw"], 8, "b_in" + L, W, tW[0], "iw_s", F32, osf, t_osf, brow=brow, t_brow=t_brow)
    P.release(m)


def phase_B2(P, C, L):
    nc, sy, d, dt = P.nc, P.sy, P.d, P.dt
    bank, tb = C["bank"], C["tb"]
    m = P.mark()
    pcur0 = P.sb("pcur0", [128, 4, 128], BF16)
    pcur = P.sb("pcur", [128, 4, 128], BF16)
    phal = P.sb("phal", [128, 4, 128], BF16)
    phal7 = P.sb("phal7", [16, 4, 128], BF16)
    poolw = P.sb("poolw", [128, 4, 128], BF16)
    psc = P.sb("psc", [128, 4], F32)
    tcn = Tile("B2c")
    sy.dma("pool", lambda e: e.dma_start(out=pcur0[:], in_=d["c_pcur0"]), reads=[dt["c_pcur0"]], writes=[tcn])
    sy.dma("pool", lambda e: e.dma_start(out=pcur[:], in_=d["c_pcur"]), reads=[dt["c_pcur"]], writes=[tcn])
    sy.dma("pool", lambda e: e.dma_start(out=phal[:], in_=d["c_phal"]), reads=[dt["c_phal"]], writes=[tcn])
    sy.dma("pool", lambda e: e.dma_start(out=phal7[:], in_=d["c_phal7"]), reads=[dt["c_phal7"]], writes=[tcn])
    sy.dma("pool", lambda e: e.dma_start(out=poolw[:], in_=d["pool_w" + L].rearrange("g c e -> c g e")), reads=[dt["pool_w" + L]], writes=[tcn])
    sy.dma("sp", lambda e: e.dma_start(out=psc[:], in_=d["c_psc" + L]), reads=[dt["c_psc" + L]], writes=[tcn])
    u = [P.sb("B2u%d" % i, [128, 512], BF16) for i in range(2)]
    uh = [P.sb("B2uh%d" % i, [128, 512], BF16) for i in range(2)]
    u7 = [P.sb("B2u7%d" % i, [16, 512], BF16) for i in range(2)]
    DT = [P.sb("B2DT%d" % i, [128, 4, 128], BF16) for i in range(2)]
    po = [P.sb("B2po%d" % i, [128, 4, 128], BF16) for i in range(2)]
    t_u = [Tile() for _ in range(2)]
    t_uh = [Tile() for _ in range(2)]
    t_u7 = [Tile() for _ in range(2)]
    t_DT = [Tile() for _ in range(2)]
    t_po = [Tile() for _ in range(2)]
    gut = d["g_ut"]
    for i in range(2):
        sy.op("dve", lambda e, i=i: e.memset(u7[i][:], 0.0), writes=[t_u7[i]])
    for j in range(NTL):
        i = j % 2
        sy.dma("sp", lambda e, j=j, i=i: e.dma_start(out=u[i][:], in_=d["up_s"][j * 128:(j + 1) * 128, :]), reads=[dt["up_s"]], writes=[t_u[i]])
        for r in range(8):
            sy.dma("sp", lambda e, j=j, i=i, r=r: e.dma_start(out=uh[i][16 * r:16 * r + 16, :], in_=gut[r, j * 16:(j + 1) * 16, :]),
                   reads=[dt["g_ut"]], writes=[t_uh[i]])
        if j >= 1:
            sy.dma("sp", lambda e, j=j, i=i: e.dma_start(out=u7[i][:], in_=gut[7, (j - 1) * 16:j * 16, :]), reads=[dt["g_ut"]], writes=[t_u7[i]])
        pc = pcur0 if j == 0 else pcur
        pD, tD = bank[0 + i], tb[0 + i]
        pM, tM = bank[2 + i], tb[2 + i]
        for g in range(4):
            o = pD[:, g * 128:(g + 1) * 128]
            sy.op("pe", lambda e, g=g, i=i, o=o, pc=pc: e.matmul(o, u[i][:, g * 128:(g + 1) * 128], pc[:, g, :], start=True, stop=False),
                  reads=[t_u[i], tcn], writes=[tD], signal=False)
            sy.op("pe", lambda e, g=g, i=i, o=o: e.matmul(o, uh[i][:, g * 128:(g + 1) * 128], phal[:, g, :], start=False, stop=False),
                  reads=[t_uh[i], tcn], writes=[tD], signal=False)
            sy.op("pe", lambda e, g=g, i=i, o=o: e.matmul(o, u7[i][0:16, g * 128:(g + 1) * 128], phal7[0:16, g, :], start=False, stop=True),
                  reads=[t_u7[i], tcn], writes=[tD], signal=(g == 3))
        sy.op("act", lambda e, i=i, pD=pD: e.activation(DT[i][:].rearrange("p g t -> p (g t)"), pD[:], AF.Copy), reads=[tD], writes=[t_DT[i]])
        for g in range(4):
            sy.op("pe", lambda e, g=g, i=i, pM=pM: e.matmul(pM[:, g * 128:(g + 1) * 128], poolw[:, g, :], DT[i][:, g, :], start=True, stop=True),
                  reads=[t_DT[i], tcn], writes=[tM], signal=(g == 3))
        for g in range(4):
            sy.op("dve", lambda e, g=g, i=i, pM=pM: e.tensor_scalar(po[i][:, g, :], pM[:, g * 128:(g + 1) * 128], psc[:, g:g + 1], None, ALU.mult),
                  reads=[tM, tcn], writes=[t_po[i]])
        dst = d["poolT_s"].rearrange("(g p) t -> p g t", p=128)[:, :, j * 128:(j + 1) * 128]
        sy.dma("sp", lambda e, i=i, dst=dst: e.dma_start(out=dst, in_=po[i][:]), reads=[t_po[i]], writes=[dt["poolT_s"]])
    P.release(m)


def phase_B3(P, C, L):
    nc, sy, d, dt = P.nc, P.sy, P.d, P.dt
    bank, tb = C["bank"], C["tb"]
    m = P.mark()
    memb = P.sb("memb", [128, 2, 1024], BF16)
    memT = P.sb("memT", [128, 8, 256], BF16)
    wkv = P.sb("wkv", [128, 8, 1024], BF16)
    mkT = P.sb("mkT", [128, 4, 256], BF16)
    mv = P.sb("mv", [128, 2, 512], BF16)
    t_memb, t_memT, t_wkv, t_mkT, t_mv = Tile(), Tile(), Tile(), Tile(), Tile()
    sy.dma("pool", lambda e: e.dma_start(out=memb[:], in_=d["mem"].rearrange("(a p) d -> p a d", p=128)), reads=[dt["mem"]], writes=[t_memb])
    sy.dma("pool", load_w(P, "w_mem_kv" + L, wkv[:], 0, 1024), reads=[dt["w_mem_kv" + L]], writes=[t_wkv])
    for a in range(2):
        for k in range(8):
            sy.op("pe", lambda e, a=a, k=k: e.transpose(C["bankb"][:, k * 128:(k + 1) * 128], memb[:, a, k * 128:(k + 1) * 128], C["ident"][:]),
                  reads=[t_memb, C["t_const"]], writes=[C["tbb"]], signal=(k == 7))
        sy.op("dve", lambda e, a=a: e.tensor_copy(memT[:, :, a * 128:(a + 1) * 128], C["bankb"][:].rearrange("p (k t) -> p k t", k=8)),
              reads=[C["tbb"]], writes=[t_memT])
    for h in range(4):
        pa, ta = bank[h % 2], tb[h % 2]
        for k in range(8):
            sy.op("pe", lambda e, h=h, k=k, pa=pa: e.matmul(pa[:, 0:256], wkv[:, k, h * 128:(h + 1) * 128], memT[:, k, :], start=(k == 0), stop=(k == 7)),
                  reads=[t_wkv, t_memT], writes=[ta], signal=(k == 7))
        sy.op("act", lambda e, h=h, pa=pa: e.activation(mkT[:, h, :], pa[:, 0:256], AF.Copy), reads=[ta], writes=[t_mkT])
    for a in range(2):
        pa, ta = bank[2 + a], tb[2 + a]
        for k in range(8):
            sy.op("pe", lambda e, a=a, k=k, pa=pa: e.matmul(pa[:], memT[:, k, a * 128:(a + 1) * 128], wkv[:, k, 512:1024], start=(k == 0), stop=(k == 7)),
                  reads=[t_wkv, t_memT], writes=[ta], signal=(k == 7))
        sy.op("act", lambda e, a=a, pa=pa: e.activation(mv[:, a, :], pa[:], AF.Copy), reads=[ta], writes=[t_mv])
    mq = [P.sb("B3mq%d" % i, [128, 4, 512], BF16) for i in range(2)]
    PT = [P.sb("B3PT%d" % i, [128, 512], BF16) for i in range(4)]
    rec = [P.sb("B3rec%d" % i, [128, 512], F32) for i in range(2)]
    mo = [P.sb("B3mo%d" % i, [128, 4, 512], BF16) for i in range(2)]
    t_mq = [Tile() for _ in range(2)]
    t_PT = [Tile() for _ in range(4)]
    t_rec = [Tile() for _ in range(2)]
    t_mo = [Tile() for _ in range(2)]
    scale = float(128.0 ** -0.5)
    for tg in range(4):
        i = tg % 2
        src = d["mqT_s"].rearrange("(h p) t -> p h t", p=128)[:, :, tg * 512:(tg + 1) * 512]
        sy.dma("sp", lambda e, i=i, src=src: e.dma_start(out=mq[i][:], in_=src), reads=[dt["mqT_s"]], writes=[t_mq[i]])
        for h in range(4):
            hh = h % 2
            for a in range(2):
                ps_, ts_ = bank[a], tb[a]
                sy.op("pe", lambda e, h=h, a=a, i=i, ps_=ps_: e.matmul(ps_[:], mkT[:, h, a * 128:(a + 1) * 128], mq[i][:, h, :], start=True, stop=True),
                      reads=[t_mkT, t_mq[i]], writes=[ts_], signal=True)
                sy.op("act", lambda e, a=a, hh=hh, ps_=ps_: e.activation(PT[2 * hh + a][:], ps_[:], AF.Exp, scale=scale), reads=[ts_], writes=[t_PT[2 * hh + a]])
            po_, to_ = bank[2 + hh], tb[2 + hh]
            pd_, td_ = bank[4 + hh], tb[4 + hh]
            for a in range(2):
                sy.op("pe", lambda e, h=h, a=a, hh=hh, po_=po_: e.matmul(po_[:], mv[:, a, h * 128:(h + 1) * 128], PT[2 * hh + a][:], start=(a == 0), stop=(a == 1)),
                      reads=[t_mv, t_PT[2 * hh + a]], writes=[to_], signal=(a == 1))
            for a in range(2):
                sy.op("pe", lambda e, a=a, hh=hh, pd_=pd_: e.matmul(pd_[:], C["ones"][:], PT[2 * hh + a][:], start=(a == 0), stop=(a == 1)),
                      reads=[C["t_const"], t_PT[2 * hh + a]], writes=[td_], signal=(a == 1))
            sy.op("dve", lambda e, hh=hh, pd_=pd_: e.reciprocal(rec[hh][:], pd_[:]), reads=[td_], writes=[t_rec[hh]])
            sy.op("dve", lambda e, h=h, hh=hh, i=i, po_=po_: e.tensor_tensor(mo[i][:, h, :], po_[:], rec[hh][:], ALU.mult),
                  reads=[to_, t_rec[hh]], writes=[t_mo[i]])
        dst = d["memT_s"].rearrange("(h p) t -> p h t", p=128)[:, :, tg * 512:(tg + 1) * 512]
        sy.dma("sp", lambda e, i=i, dst=dst: e.dma_start(out=dst, in_=mo[i][:]), reads=[t_mo[i]], writes=[dt["memT_s"]])
    P.release(m)


def phase_B4(P, C, L):
    nc, sy, d, dt = P.nc, P.sy, P.d, P.dt
    bank, tb = C["bank"], C["tb"]
    m = P.mark()
    ikT2 = P.sb("ikT2", [128, S], BF16)
    scores = P.sb("scores", [128, S], F32)
    junk = P.sb("junk", [128, S // 2], U8)
    cnt2 = P.sb("B4cnt2", [128, 1], F32)
    maskt = P.sb("maskt", [128, 1024], F32)
    t_ik, t_sc, t_junk, t_mt = Tile(), Tile(), Tile(), Tile()
    gik = d["g_ik"]
    for half in range(2):
        for r in range(8):
            dst = ikT2[64 * half:64 * half + 64, :].rearrange("p (j r t) -> p j r t", j=NTL, r=8)[:, :, r, :]
            src = gik[r].rearrange("p (j t) -> p j t", j=NTL)
            sy.dma("sp", lambda e, dst=dst, src=src: e.dma_start(out=dst, in_=src), reads=[dt["g_ik"]], writes=[t_ik])
    sy.dma("sp", lambda e: e.dma_start(out=maskt[:], in_=d["c_mask"]), reads=[dt["c_mask"]], writes=[t_mt])
    iq = [P.sb("B4iq%d" % i, [128, 4, 128], BF16) for i in range(2)]
    qq = [P.sb("B4q%d" % i, [128, 4, 128], BF16) for i in range(2)]
    iw = [P.sb("B4iw%d" % i, [128, 8], F32) for i in range(2)]
    diag = [P.sb("B4dg%d" % i, [128, 8, 128], BF16) for i in range(2)]
    t_iq = [Tile() for _ in range(2)]
    t_qq = [Tile() for _ in range(2)]
    t_iw = [Tile() for _ in range(2)]
    t_dg = [Tile() for _ in range(2)]
    R = [P.sb("B4R%d" % i, [128, 512], BF16) for i in range(4)]
    t_R = [Tile() for _ in range(4)]
    cnt = P.sb("B4cnt", [128, 1], F32)
    tmpc = P.sb("B4tmp", [128, 1], F32)
    mid = [P.sb("B4mid%d" % i, [128, 1], F32) for i in range(2)]
    thr = P.sb("B4thr", [128, 1], F32)
    t_bis = Tile()
    Mk = [P.sb("B4M%d" % i, [128, 512], BF16) for i in range(2)]
    MT = [P.sb("B4MT%d" % i, [128, 512], BF16) for i in range(2)]
    t_Mk = [Tile() for _ in range(2)]
    t_MT = [Tile() for _ in range(2)]
    Kt = [P.sb("B4K%d" % i, [128, 4, 512], BF16) for i in range(2)]
    Vs = [P.sb("B4Vs%d" % i, [128, 4, 512], BF16) for i in range(2)]
    Vx = [P.sb("B4Vx%d" % i, [128, 4, 8, 128], BF16) for i in range(2)]
    t_K = [Tile() for _ in range(2)]
    t_Vs = [Tile() for _ in range(2)]
    t_Vx = [Tile() for _ in range(2)]
    E = [P.sb("B4E%d" % i, [128, 512], BF16) for i in range(2)]
    PTt = E
    t_E = [Tile() for _ in range(2)]
    t_PT = [Tile() for _ in range(2)]
    rec = P.sb("B4rec", [64, 8, 128], F32)
    oo = P.sb("B4oo", [64, 8, 128], BF16)
    dsab = [P.sb("B4dsab%d" % i, [128, 4, 128], BF16) for i in range(2)]
    t_rec, t_oo = Tile(), Tile()
    t_dsab = [Tile() for _ in range(2)]
    for i in range(2):
        sy.op("pool", lambda e, i=i: e.memset(Vx[i][:], 1.0), writes=[t_Vx[i]])
    gk = d["g_k"]
    gv = d["g_v"]
    qsrc = d["qT_s"].rearrange("(c p) t -> p c t", p=128)
    iqsrc = d["iqT_s"].rearrange("(c p) t -> p c t", p=128)
    acc = [bank[4], bank[5]]
    t_acc = [tb[4], tb[5]]
    kv_i = 0
    for j in range(NTL):
        b = j % 2
        ntile = 2 * (j + 1)
        Sj = 1024 * (j + 1)
        sy.dma("sp", lambda e, j=j, b=b: e.dma_start(out=iq[b][:], in_=iqsrc[:, :, j * 128:(j + 1) * 128]), reads=[dt["iqT_s"]], writes=[t_iq[b]])
        sy.dma("sp", lambda e, j=j, b=b: e.dma_start(out=qq[b][:], in_=qsrc[:, :, j * 128:(j + 1) * 128]), reads=[dt["qT_s"]], writes=[t_qq[b]])
        sy.dma("sp", lambda e, j=j, b=b: e.dma_start(out=iw[b][:], in_=d["iw_s"][j * 128:(j + 1) * 128, :]), reads=[dt["iw_s"]], writes=[t_iw[b]])
        for h in range(8):
            sy.op("pool", lambda e, h=h, b=b: e.tensor_scalar(diag[b][:, h, :], C["ident"][:], iw[b][:, h:h + 1], None, ALU.mult),
                  reads=[t_iw[b], C["t_const"]], writes=[t_dg[b]])
        for i in range(ntile):
            psc_, tsc_ = bank[2 + i % 2], tb[2 + i % 2]
            def emit_x(h, i=i, b=b):
                px, tx = bank[h % 2], tb[h % 2]
                hb = 64 * (h % 2)
                sy.op("pe", lambda e, h=h, i=i, b=b, px=px, hb=hb: e.matmul(px[:], iq[b][hb:hb + 64, h // 2, :], ikT2[hb:hb + 64, i * 512:(i + 1) * 512], start=True, stop=True),
                      reads=[t_iq[b], t_ik], writes=[tx], signal=True)
            emit_x(0)
            for h in range(8):
                px, tx = bank[h % 2], tb[h % 2]
                ri = (i * 8 + h) % 4
                if h + 1 < 8:
                    emit_x(h + 1)
                if h % 2 == 0:
                    sy.op("act", lambda e, px=px, ri=ri: e.activation(R[ri][:], px[:], AF.Relu), reads=[tx], writes=[t_R[ri]])
                else:
                    sy.op("dve", lambda e, px=px, ri=ri: e.tensor_scalar(R[ri][:], px[:], 0.0, None, ALU.max), reads=[tx], writes=[t_R[ri]])
                sy.op("pe", lambda e, h=h, b=b, psc_=psc_, ri=ri: e.matmul(psc_[:], diag[b][:, h, :], R[ri][:], start=(h == 0), stop=(h == 7)),
                      reads=[t_dg[b], t_R[ri]], writes=[tsc_], signal=(h == 7))
            if i >= ntile - 2:
                mo = (i - (ntile - 2)) * 512
                sy.op("dve", lambda e, i=i, psc_=psc_, mo=mo: e.tensor_tensor(scores[:, i * 512:(i + 1) * 512], psc_[:], maskt[:, mo:mo + 512], ALU.add),
                      reads=[tsc_, t_mt], writes=[t_sc])
            else:
                sy.op("act", lambda e, i=i, psc_=psc_: e.activation(scores[:, i * 512:(i + 1) * 512], psc_[:], AF.Copy), reads=[tsc_], writes=[t_sc])
        sy.op("dve", lambda e: e.memset(mid[0][:], 0.0), writes=[t_bis])
        for it in range(NIT):
            a, bb = it % 2, (it + 1) % 2
            step = BIS_W / (2.0 ** (it + 1))
            if Sj <= S // 2:
                sy.op("dve", lambda e, a=a, Sj=Sj: e.tensor_scalar(junk[:, 0:Sj], scores[:, 0:Sj], mid[a][:, 0:1], None, ALU.is_ge, ALU.add, accum_out=cnt[:, 0:1]),
                      reads=[t_sc, t_bis], writes=[t_junk, t_bis])
            else:
                H = S // 2
                sy.op("dve", lambda e, a=a: e.tensor_scalar(junk[:, 0:H], scores[:, 0:H], mid[a][:, 0:1], None, ALU.is_ge, ALU.add, accum_out=cnt2[:, 0:1]),
                      reads=[t_sc, t_bis], writes=[t_junk, t_bis])
                sy.op("dve", lambda e, a=a, Sj=Sj: e.tensor_scalar(junk[:, 0:Sj - H], scores[:, H:Sj], mid[a][:, 0:1], cnt2[:, 0:1], ALU.is_ge, ALU.add, accum_out=cnt[:, 0:1]),
                      reads=[t_sc, t_bis], writes=[t_junk, t_bis])
            sy.op("dve", lambda e, step=step: e.tensor_scalar(tmpc[:], cnt[:], 255.5, float(2.0 * step), ALU.is_ge, ALU.mult), reads=[t_bis], writes=[t_bis])
            sy.op("dve", lambda e, a=a, bb=bb, step=step: e.scalar_tensor_tensor(mid[bb][:], tmpc[:], float(-step), mid[a][:], ALU.add, ALU.add),
                  reads=[t_bis], writes=[t_bis])
        laststep = BIS_W / (2.0 ** NIT)
        sy.op("dve", lambda e: e.tensor_scalar(thr[:], mid[NIT % 2][:], float(-laststep), None, ALU.add), reads=[t_bis], writes=[t_bis])
        for i in range(ntile):
            kb = kv_i % 2
            kv_i += 1
            jj, r0 = i // 2, 4 * (i % 2)
            ksrc = gk[r0:r0 + 4, :, :, jj * 128:(jj + 1) * 128].rearrange("r p c t -> p c r t")
            vsrc = gv[r0:r0 + 4, jj * 128:(jj + 1) * 128, :].rearrange("r t d -> t r d")
            sy.dma("sp", lambda e, kb=kb, ksrc=ksrc: e.dma_start(out=Kt[kb][:].rearrange("p c (r t) -> p c r t", r=4), in_=ksrc), reads=[dt["g_k"]], writes=[t_K[kb]])
            sy.dma("sp", lambda e, kb=kb, vsrc=vsrc: e.dma_start(out=Vs[kb][:], in_=vsrc), reads=[dt["g_v"]], writes=[t_Vs[kb]])
            sy.op("pool", lambda e, kb=kb: e.tensor_copy(Vx[kb][:, :, :, 0:64], Vs[kb][:].rearrange("p c (h d) -> p c h d", h=8)),
                  reads=[t_Vs[kb]], writes=[t_Vx[kb]])
            sy.op("dve", lambda e, i=i, kb=kb: e.tensor_scalar(Mk[kb][:], scores[:, i * 512:(i + 1) * 512], thr[:, 0:1], None, ALU.is_ge),
                  reads=[t_sc, t_bis], writes=[t_Mk[kb]])
            for c in range(4):
                sy.op("pe", lambda e, c=c, kb=kb: e.transpose(C["bankb"][:, c * 128:(c + 1) * 128], Mk[kb][:, c * 128:(c + 1) * 128], C["ident"][:]),
                      reads=[t_Mk[kb], C["t_const"]], writes=[C["tbb"]], signal=(c == 3))
            sy.op("act", lambda e, kb=kb: e.activation(MT[kb][:], C["bankb"][:, 0:512], AF.Copy), reads=[C["tbb"]], writes=[t_MT[kb]])
            def emit_st(h, kb=kb, b=b):
                pst, tst = bank[h % 2], tb[h % 2]
                hb = 64 * (h % 2)
                for c in range(4):
                    sy.op("pe", lambda e, h=h, c=c, kb=kb, b=b, pst=pst, hb=hb: e.matmul(pst[:, c * 128:(c + 1) * 128], Kt[kb][hb:hb + 64, h // 2, c * 128:(c + 1) * 128], qq[b][hb:hb + 64, h // 2, :], start=True, stop=True),
                          reads=[t_K[kb], t_qq[b]], writes=[tst], signal=(c == 3))
            emit_st(0)
            for h in range(8):
                pst, tst = bank[h % 2], tb[h % 2]
                eb = h % 2
                if h + 1 < 8:
                    emit_st(h + 1)
                sy.op("act", lambda e, pst=pst, eb=eb: e.activation(E[eb][:], pst[:], AF.Exp, scale=0.125), reads=[tst], writes=[t_E[eb]])
                sy.op("dve", lambda e, eb=eb, kb=kb: e.tensor_tensor(PTt[eb][:], E[eb][:], MT[kb][:], ALU.mult), reads=[t_E[eb], t_MT[kb]], writes=[t_E[eb]])
                ab = h // 4
                for c in range(4):
                    first = (i == 0 and c == 0 and h % 4 == 0)
                    last = (i == ntile - 1 and c == 3)
                    sy.op("pe", lambda e, h=h, c=c, kb=kb, eb=eb, ab=ab, first=first, last=last: e.matmul(
                        acc[ab][:, (h % 4) * 128:(h % 4 + 1) * 128], Vx[kb][:, c, h, :], PTt[eb][:, c * 128:(c + 1) * 128],
                        start=first, stop=last, skip_group_check=True),
                        reads=[t_Vx[kb], t_E[eb]], writes=[t_acc[ab]], signal=(c == 3))
        for ab in range(2):
            sy.op("dve", lambda e, ab=ab: e.reciprocal(rec[0:64, 4 * ab:4 * ab + 4, :].rearrange("p h t -> p (h t)"), acc[ab][64:128, :]),
                  reads=[t_acc[ab]], writes=[t_rec])
            sy.op("dve", lambda e, ab=ab: e.tensor_tensor(oo[0:64, 4 * ab:4 * ab + 4, :].rearrange("p h t -> p (h t)"), acc[ab][0:64, :],
                                                          rec[0:64, 4 * ab:4 * ab + 4, :].rearrange("p h t -> p (h t)"), ALU.mult),
                  reads=[t_acc[ab], t_rec], writes=[t_oo])
        oov = oo[:].rearrange("p (a two) t -> p a two t", two=2)
        sy.op("dve", lambda e, b=b, oov=oov: e.tensor_copy(dsab[b][0:64, :, :], oov[0:64, :, 0, :]), reads=[t_oo], writes=[t_dsab[b]])
        sy.op("dve", lambda e, b=b, oov=oov: e.tensor_copy(dsab[b][64:128, :, :], oov[0:64, :, 1, :]), reads=[t_oo], writes=[t_dsab[b]])
        dst = d["dsaT_s"].rearrange("(a p) t -> p a t", p=128)[:, :, j * 128:(j + 1) * 128]
        sy.dma("sp", lambda e, b=b, dst=dst: e.dma_start(out=dst, in_=dsab[b][:]), reads=[t_dsab[b]], writes=[dt["dsaT_s"]])
    P.release(m)


def layer_norm_tile(P, C, y, t_y, stats, mv, sd, rstd, xn, gam, bet, out, t_tmp, t_ln, t_out):
    sy = P.sy
    for hlf in range(2):
        sy.op("dve", lambda e, hlf=hlf: e.bn_stats(stats[:, hlf * 6:(hlf + 1) * 6], y[:, hlf * 512:(hlf + 1) * 512]), reads=[t_y], writes=[t_tmp])
    sy.op("dve", lambda e: e.bn_aggr(mv[:], stats[:]), reads=[t_tmp], writes=[t_tmp])
    sy.op("act", lambda e: e.activation(sd[:], mv[:, 1:2], AF.Sqrt, bias=C["eps"][:, 0:1]), reads=[t_tmp, C["t_const"]], writes=[t_tmp])
    sy.op("dve", lambda e: e.reciprocal(rstd[:], sd[:]), reads=[t_tmp], writes=[t_tmp])
    sy.op("dve", lambda e: e.tensor_scalar(xn[:], y[:], mv[:, 0:1], rstd[:, 0:1], ALU.subtract, ALU.mult), reads=[t_y, t_tmp], writes=[t_tmp])
    sy.op("pool", lambda e: e.tensor_tensor(xn[:], xn[:], gam[:], ALU.mult), reads=[t_tmp, t_ln], writes=[t_tmp])
    sy.op("pool", lambda e: e.tensor_tensor(out[:], xn[:], bet[:], ALU.add), reads=[t_tmp, t_ln], writes=[t_out])


def phase_B5(P, C, L, xname):
    nc, sy, d, dt = P.nc, P.sy, P.d, P.dt
    bank, tb = C["bank"], C["tb"]
    xT, t_xT = C["xT"], C["t_xT"]
    x2T, t_x2T = C["x2T"], C["t_x2T"]
    m = P.mark()
    wbr = P.sb("wbr", [128, 3, 4, 1024], BF16)
    wout = P.sb("wout", [128, 8, 1024], BF16)
    wg = [P.sb("wg%d" % i, [128, 8, 1024], BF16) for i in range(1)] * 2
    wr = P.sb("wr", [128, 8, 16], F32)
    gam = P.sb("gam", [128, 1024], F32)
    bet = P.sb("bet", [128, 1024], F32)
    t_w, t_ln = Tile(), Tile()
    t_wg = [Tile()] * 2
    for n in range(3):
        sy.dma("pool", lambda e, n=n: e.dma_start(out=wbr[:, n, :, :], in_=d["w_br" + L][n].rearrange("(a c) d -> c a d", c=128)), reads=[dt["w_br" + L]], writes=[t_w])
    sy.dma("pool", load_w(P, "w_out" + L, wout[:], 0, 1024), reads=[dt["w_out" + L]], writes=[t_w])
    sy.dma("sp", lambda e: e.dma_start(out=wr[:], in_=d["w_router"].rearrange("(k p) e -> p k e", p=128)), reads=[dt["w_router"]], writes=[t_w])
    sy.dma("sp", lambda e: e.dma_start(out=gam[:], in_=bcast_rows(d["ln1_g" + L], 128)), reads=[dt["ln1_g" + L]], writes=[t_ln])
    sy.dma("sp", lambda e: e.dma_start(out=bet[:], in_=bcast_rows(d["ln1_b" + L], 128)), reads=[dt["ln1_b" + L]], writes=[t_ln])
    br = [P.sb("B5br%d" % i, [128, 4, 512], BF16) for i in range(2)]
    t_br = [Tile() for _ in range(2)]
    gt = [P.sb("B5g%d" % i, [128, 512], F32) for i in range(2)]
    t_gt = [Tile() for _ in range(2)]
    pg = [P.sb("B5pg%d" % i, [128, 512], F32) for i in range(2)]
    t_pg = [Tile() for _ in range(2)]
    mTf = P.sb("B5mTf", [128, 8, 512], F32)
    mTb = P.sb("B5mTb", [128, 8, 512], BF16)
    t_mTf, t_mTb = Tile(), Tile()
    xt = [P.sb("B5x%d" % i, [128, 1024], F32) for i in range(2)]
    t_xt = [Tile() for _ in range(2)]
    y = P.sb("B5y", [128, 1024], F32)
    xn = P.sb("B5xn", [128, 1024], F32)
    x1 = [P.sb("B5x1%d" % i, [128, 1024], F32) for i in range(2)]
    x1b = P.sb("B5x1b", [128, 1024], BF16)
    x1Tf = P.sb("B5x1Tf", [128, 8, 128], F32)
    stats = P.sb("B5st", [128, 12], F32)
    mv = P.sb("B5mv", [128, 2], F32)
    sd = P.sb("B5sd", [128, 1], F32)
    rstd = P.sb("B5rstd", [128, 1], F32)
    t_y, t_tmp, t_x1b, t_x1Tf = Tile(), Tile(), Tile(), Tile()
    t_x1 = [Tile() for _ in range(2)]
    names = ["poolT_s", "dsaT_s", "memT_s"]
    bcg = C["bc_gate"]
    li = 0
    for tg in range(4):
        for n in range(3):
            bi = li % 2
            li += 1
            src = d[names[n]].rearrange("(a p) t -> p a t", p=128)[:, :, tg * 512:(tg + 1) * 512]
            sy.dma("sp", lambda e, bi=bi, src=src: e.dma_start(out=br[bi][:], in_=src), reads=[dt[names[n]]], writes=[t_br[bi]])
            sy.dma("pool", load_w(P, "w_in" + L, wg[bi][:], OFF["gate"] + n * 1024, 1024), reads=[dt["w_in" + L]], writes=[t_wg[bi]])
            for dc in range(8):
                pp, tp = bank[dc % 2], tb[dc % 2]
                pq, tq = bank[2 + dc % 2], tb[2 + dc % 2]
                gi = dc % 2
                for a in range(4):
                    sy.op("pe", lambda e, n=n, a=a, dc=dc, bi=bi, pp=pp: e.matmul(pp[:], wbr[:, n, a, dc * 128:(dc + 1) * 128], br[bi][:, a, :], start=(a == 0), stop=(a == 3)),
                          reads=[t_w, t_br[bi]], writes=[tp], signal=(a == 3))
                for k in range(8):
                    sy.op("pe", lambda e, k=k, dc=dc, bi=bi, pq=pq, tg=tg: e.matmul(pq[:], wg[bi][:, k, dc * 128:(dc + 1) * 128], xT[:, k, tg * 512:(tg + 1) * 512], start=(k == 0), stop=(k == 7)),
                          reads=[t_wg[bi], t_xT], writes=[tq], signal=(k == 7))
                sy.op("act", lambda e, n=n, dc=dc, gi=gi, pq=pq: e.activation(gt[gi][:], pq[:], AF.Sigmoid, bias=C["bcols"][:, bcg + n * 8 + dc:bcg + n * 8 + dc + 1]),
                      reads=[tq, C["t_const"]], writes=[t_gt[gi]])
                if n == 0:
                    sy.op("dve", lambda e, dc=dc, gi=gi, pp=pp: e.tensor_tensor(mTf[:, dc, :], pp[:], gt[gi][:], ALU.mult), reads=[tp, t_gt[gi]], writes=[t_mTf])
                else:
                    sy.op("dve", lambda e, gi=gi, pp=pp: e.tensor_tensor(pg[gi][:], pp[:], gt[gi][:], ALU.mult), reads=[tp, t_gt[gi]], writes=[t_pg[gi]])
                    if n == 1:
                        sy.op("pool", lambda e, dc=dc, gi=gi: e.tensor_tensor(mTf[:, dc, :], mTf[:, dc, :], pg[gi][:], ALU.add), reads=[t_pg[gi], t_mTf], writes=[t_mTf])
                    else:
                        sy.op("pool", lambda e, dc=dc, gi=gi: e.tensor_tensor(mTb[:, dc, :], mTf[:, dc, :], pg[gi][:], ALU.add), reads=[t_pg[gi], t_mTf], writes=[t_mTb])
        for tl in range(4):
            j = tg * 4 + tl
            xi = j % 2
            sy.dma("sp", lambda e, j=j, xi=xi: e.dma_start(out=xt[xi][:], in_=d[xname][j * 128:(j + 1) * 128, :]), reads=[dt[xname]], writes=[t_xt[xi]])
            for hlf in range(2):
                pm, tm = bank[4 + hlf], tb[4 + hlf]
                for dc in range(8):
                    sy.op("pe", lambda e, dc=dc, hlf=hlf, tl=tl, pm=pm: e.matmul(pm[:], mTb[:, dc, tl * 128:(tl + 1) * 128], wout[:, dc, hlf * 512:(hlf + 1) * 512], start=(dc == 0), stop=(dc == 7)),
                          reads=[t_mTb, t_w], writes=[tm], signal=(dc == 7))
                sy.op("dve", lambda e, hlf=hlf, xi=xi, pm=pm: e.scalar_tensor_tensor(y[:, hlf * 512:(hlf + 1) * 512], xt[xi][:, hlf * 512:(hlf + 1) * 512], float(ALPHA), pm[:], ALU.mult, ALU.add),
                      reads=[tm, t_xt[xi]], writes=[t_y])
            layer_norm_tile(P, C, y, t_y, stats, mv, sd, rstd, xn, gam, bet, x1[xi], t_tmp, t_ln, t_x1[xi])
            sy.dma("sp", lambda e, j=j, xi=xi: e.dma_start(out=d["x1_s"][j * 128:(j + 1) * 128, :], in_=x1[xi][:]), reads=[t_x1[xi]], writes=[dt["x1_s"]])
            sy.op("act", lambda e, xi=xi: e.activation(x1b[:], x1[xi][:], AF.Copy), reads=[t_x1[xi]], writes=[t_x1b])
            for k in range(8):
                sy.op("pe", lambda e, k=k: e.transpose(C["bankb"][:, k * 128:(k + 1) * 128], x1b[:, k * 128:(k + 1) * 128], C["ident"][:]),
                      reads=[t_x1b, C["t_const"]], writes=[C["tbb"]], signal=(k == 7))
            sy.op("dve", lambda e, j=j: e.tensor_copy(x2T[:, :, j * 128:(j + 1) * 128], C["bankb"][:].rearrange("p (k t) -> p k t", k=8)),
                  reads=[C["tbb"]], writes=[t_x2T])
            for hlf in range(2):
                pf, tf = bank[hlf], tb[hlf]
                for kk in range(4):
                    k = hlf * 4 + kk
                    sy.op("pe", lambda e, k=k, kk=kk, xi=xi, pf=pf: e.transpose(pf[:, kk * 128:(kk + 1) * 128], x1[xi][:, k * 128:(k + 1) * 128], C["identf"][:]),
                          reads=[t_x1[xi], C["t_const"]], writes=[tf], signal=(kk == 3))
                sy.op("act", lambda e, hlf=hlf, pf=pf: e.activation(x1Tf[:, hlf * 4:(hlf + 1) * 4, :].rearrange("p k t -> p (k t)"), pf[:], AF.Copy), reads=[tf], writes=[t_x1Tf])
            pl, tl_ = bank[6], tb[6]
            for k in range(8):
                sy.op("pe", lambda e, k=k, pl=pl: e.matmul(pl[:, 0:16], x1Tf[:, k, :], wr[:, k, :], start=(k == 0), stop=(k == 7)),
                      reads=[t_x1Tf, t_w], writes=[tl_], signal=(k == 7))
            sy.op("act", lambda e, j=j, pl=pl: e.activation(C["logits"][:, j, :], pl[:, 0:16], AF.Copy), reads=[tl_], writes=[C["t_logits"]])
    P.release(m)


def phase_B6(P, C):
    nc, sy, d, dt = P.nc, P.sy, P.d, P.dt
    m = P.mark()
    t = Tile("router")
    lg = C["logits"]
    gate = C["gate"]
    BIG = 1.0e9
    rb = P.sb("R_rb", [128, 16], F32)
    sc = P.sb("R_sc", [128, 16, 16], F32)
    bi = P.sb("R_bi", [128, 16, 16], F32)
    b2 = P.sb("R_b2", [128, 16, 16], F32)
    eq = P.sb("R_eq", [128, 16, 16], F32)
    m1 = P.sb("R_m1", [128, 64], F32)
    m2 = P.sb("R_m2", [128, 64], F32)
    gs = P.sb("R_gs", [128, 64], F32)
    gmax = P.sb("R_gmax", [128, 16], F32)
    pen = P.sb("R_pen", [128, 64], F32)
    s1 = P.sb("R_s1", [128, 16], F32)
    s2 = P.sb("R_s2", [128, 16], F32)
    den = P.sb("R_den", [128, 16], F32)
    sy.dma("sp", lambda e: e.dma_start(out=rb[:], in_=bcast_rows(d["router_bias"], 128)), reads=[dt["router_bias"]], writes=[t])
    sy.op("act", lambda e: e.activation(sc[:], lg[:], AF.Sigmoid), reads=[C["t_logits"]], writes=[t])
    for j in range(NTL):
        sy.op("dve", lambda e, j=j: e.tensor_tensor(bi[:, j, :], sc[:, j, :], rb[:], ALU.add), reads=[t], writes=[t])
    v4 = lambda x: x[:].rearrange("p t (g e) -> p (t g) e", g=4)
    b64 = lambda x: x[:].unsqueeze(2).to_broadcast([128, 64, 4])
    b16 = lambda x: x[:].unsqueeze(2).to_broadcast([128, 16, 16])
    sy.op("dve", lambda e: e.tensor_reduce(m1[:], v4(bi), AX.X, ALU.max), reads=[t], writes=[t])
    sy.op("dve", lambda e: e.tensor_tensor(v4(eq), v4(bi), b64(m1), ALU.is_equal), reads=[t], writes=[t])
    sy.op("dve", lambda e: e.scalar_tensor_tensor(b2[:].rearrange("p t e -> p (t e)"), eq[:].rearrange("p t e -> p (t e)"), -BIG, bi[:].rearrange("p t e -> p (t e)"), ALU.mult, ALU.add), reads=[t], writes=[t])
    sy.op("dve", lambda e: e.tensor_reduce(m2[:], v4(b2), AX.X, ALU.max), reads=[t], writes=[t])
    sy.op("dve", lambda e: e.tensor_tensor(gs[:], m1[:], m2[:], ALU.add), reads=[t], writes=[t])
    sy.op("dve", lambda e: e.tensor_reduce(gmax[:], gs[:].rearrange("p (t g) -> p t g", g=4), AX.X, ALU.max), reads=[t], writes=[t])
    sy.op("dve", lambda e: e.tensor_tensor(pen[:].rearrange("p (t g) -> p t g", g=4), gs[:].rearrange("p (t g) -> p t g", g=4),
                                            gmax[:].unsqueeze(2).to_broadcast([128, 16, 4]), ALU.is_equal), reads=[t], writes=[t])
    sy.op("dve", lambda e: e.tensor_scalar(pen[:], pen[:], BIG, -BIG, ALU.mult, ALU.add), reads=[t], writes=[t])
    sy.op("dve", lambda e: e.tensor_tensor(v4(b2), v4(bi), b64(pen), ALU.add), reads=[t], writes=[t])
    sy.op("dve", lambda e: e.tensor_reduce(s1[:], b2[:], AX.X, ALU.max), reads=[t], writes=[t])
    sy.op("dve", lambda e: e.tensor_tensor(eq[:], b2[:], b16(s1), ALU.is_equal), reads=[t], writes=[t])
    sy.op("dve", lambda e: e.scalar_tensor_tensor(bi[:].rearrange("p t e -> p (t e)"), eq[:].rearrange("p t e -> p (t e)"), -BIG, b2[:].rearrange("p t e -> p (t e)"), ALU.mult, ALU.add), reads=[t], writes=[t])
    sy.op("dve", lambda e: e.tensor_reduce(s2[:], bi[:], AX.X, ALU.max), reads=[t], writes=[t])
    sy.op("dve", lambda e: e.tensor_tensor(eq[:], b2[:], b16(s2), ALU.is_ge), reads=[t], writes=[t])
    sy.op("dve", lambda e: e.tensor_tensor(b2[:], eq[:], sc[:], ALU.mult), reads=[t], writes=[t])
    sy.op("dve", lambda e: e.tensor_reduce(den[:], b2[:], AX.X, ALU.add), reads=[t], writes=[t])
    sy.op("dve", lambda e: e.reciprocal(den[:], den[:]), reads=[t], writes=[t])
    sy.op("dve", lambda e: e.tensor_tensor(gate[:], b2[:], b16(den), ALU.mult), reads=[t], writes=[C["t_gate"]])
    P.release(m)


def phase_B7(P, C, L):
    nc, sy, d, dt = P.nc, P.sy, P.d, P.dt
    bank, tb = C["bank"], C["tb"]
    x2T, t_x2T = C["x2T"], C["t_x2T"]
    acc, t_acc = C["acc"], C["t_acc"]
    gate = C["gate"]
    m = P.mark()
    w1 = [P.sb("w1_%d" % i, [128, 8, 512], BF16) for i in range(2)]
    w3 = [P.sb("w3_%d" % i, [128, 8, 512], BF16) for i in range(2)]
    w2 = [P.sb("w2_%d" % i, [128, 4, 1024], BF16) for i in range(2)]
    t_we = [Tile() for _ in range(2)]
    sl = [P.sb("B7s%d" % i, [128, 512], F32) for i in range(2)]
    t_sl = [Tile() for _ in range(2)]
    hT = [P.sb("B7h%d" % i, [128, 4, 512], BF16) for i in range(2)]
    t_hT = [Tile() for _ in range(2)]
    hi = 0
    for ex in range(16):
        wb = ex % 2
        sy.dma("pool", lambda e, ex=ex, wb=wb: e.dma_start(out=w1[wb][:], in_=d["w1" + L][ex].rearrange("(k p) f -> p k f", p=128)), reads=[dt["w1" + L]], writes=[t_we[wb]])
        sy.dma("pool", lambda e, ex=ex, wb=wb: e.dma_start(out=w3[wb][:], in_=d["w3" + L][ex].rearrange("(k p) f -> p k f", p=128)), reads=[dt["w3" + L]], writes=[t_we[wb]])
        sy.dma("pool", lambda e, ex=ex, wb=wb: e.dma_start(out=w2[wb][:], in_=d["w2" + L][ex].rearrange("(k p) f -> p k f", p=128)), reads=[dt["w2" + L]], writes=[t_we[wb]])
        for tg in range(4):
            hb = hi % 2
            hi += 1
            for fc in range(4):
                p1, t1 = bank[fc % 2], tb[fc % 2]
                p3, t3 = bank[2 + fc % 2], tb[2 + fc % 2]
                si = fc % 2
                for k in range(8):
                    sy.op("pe", lambda e, k=k, fc=fc, wb=wb, tg=tg, p1=p1: e.matmul(p1[:], w1[wb][:, k, fc * 128:(fc + 1) * 128], x2T[:, k, tg * 512:(tg + 1) * 512], start=(k == 0), stop=(k == 7)),
                          reads=[t_we[wb], t_x2T], writes=[t1], signal=(k == 7))
                for k in range(8):
                    sy.op("pe", lambda e, k=k, fc=fc, wb=wb, tg=tg, p3=p3: e.matmul(p3[:], w3[wb][:, k, fc * 128:(fc + 1) * 128], x2T[:, k, tg * 512:(tg + 1) * 512], start=(k == 0), stop=(k == 7)),
                          reads=[t_we[wb], t_x2T], writes=[t3], signal=(k == 7))
                sy.op("act", lambda e, si=si, p1=p1: e.activation(sl[si][:], p1[:], AF.Silu), reads=[t1], writes=[t_sl[si]])
                sy.op("dve", lambda e, fc=fc, hb=hb, si=si, p3=p3: e.tensor_tensor(hT[hb][:, fc, :], p3[:], sl[si][:], ALU.mult), reads=[t3, t_sl[si]], writes=[t_hT[hb]])
            for tl in range(4):
                j = tg * 4 + tl
                for hlf in range(2):
                    py, ty = bank[4 + hlf], tb[4 + hlf]
                    for fc in range(4):
                        sy.op("pe", lambda e, fc=fc, hb=hb, tl=tl, wb=wb, hlf=hlf, py=py: e.matmul(py[:], hT[hb][:, fc, tl * 128:(tl + 1) * 128], w2[wb][:, fc, hlf * 512:(hlf + 1) * 512], start=(fc == 0), stop=(fc == 3)),
                              reads=[t_hT[hb], t_we[wb]], writes=[ty], signal=(fc == 3))
                    if ex == 0:
                        sy.op("dve", lambda e, j=j, hlf=hlf, py=py, ex=ex: e.tensor_scalar(acc[:, j, hlf * 512:(hlf + 1) * 512], py[:], gate[:, j, ex:ex + 1], None, ALU.mult),
                              reads=[ty, C["t_gate"]], writes=[t_acc])
                    else:
                        sy.op("dve", lambda e, j=j, hlf=hlf, py=py, ex=ex: e.scalar_tensor_tensor(acc[:, j, hlf * 512:(hlf + 1) * 512], py[:], gate[:, j, ex:ex + 1], acc[:, j, hlf * 512:(hlf + 1) * 512], ALU.mult, ALU.add),
                              reads=[ty, C["t_gate"], t_acc], writes=[t_acc])
    P.release(m)


def phase_B8(P, C, L, out_name):
    nc, sy, d, dt = P.nc, P.sy, P.d, P.dt
    acc, t_acc = C["acc"], C["t_acc"]
    m = P.mark()
    gam = P.sb("gam2", [128, 1024], F32)
    bet = P.sb("bet2", [128, 1024], F32)
    t_ln = Tile()
    sy.dma("sp", lambda e: e.dma_start(out=gam[:], in_=bcast_rows(d["ln2_g" + L], 128)), reads=[dt["ln2_g" + L]], writes=[t_ln])
    sy.dma("sp", lambda e: e.dma_start(out=bet[:], in_=bcast_rows(d["ln2_b" + L], 128)), reads=[dt["ln2_b" + L]], writes=[t_ln])
    xt = [P.sb("B8x%d" % i, [128, 1024], F32) for i in range(2)]
    t_xt = [Tile() for _ in range(2)]
    y = P.sb("B8y", [128, 1024], F32)
    xn = P.sb("B8xn", [128, 1024], F32)
    xo = [P.sb("B8xo%d" % i, [128, 1024], F32) for i in range(2)]
    t_xo = [Tile() for _ in range(2)]
    stats = P.sb("B8st", [128, 12], F32)
    mv = P.sb("B8mv", [128, 2], F32)
    sd = P.sb("B8sd", [128, 1], F32)
    rstd = P.sb("B8rstd", [128, 1], F32)
    t_y, t_tmp = Tile(), Tile()
    for j in range(NTL):
        xi = j % 2
        sy.dma("sp", lambda e, j=j, xi=xi: e.dma_start(out=xt[xi][:], in_=d["x1_s"][j * 128:(j + 1) * 128, :]), reads=[dt["x1_s"]], writes=[t_xt[xi]])
        sy.op("dve", lambda e, j=j, xi=xi: e.scalar_tensor_tensor(y[:], xt[xi][:], float(ALPHA), acc[:, j, :], ALU.mult, ALU.add),
              reads=[t_xt[xi], t_acc], writes=[t_y])
        layer_norm_tile(P, C, y, t_y, stats, mv, sd, rstd, xn, gam, bet, xo[xi], t_tmp, t_ln, t_xo[xi])
        sy.dma("sp", lambda e, j=j, xi=xi: e.dma_start(out=d[out_name][j * 128:(j + 1) * 128, :], in_=xo[xi][:]), reads=[t_xo[xi]], writes=[dt[out_name]])
    P.release(m)


BCOLS = {}
_o = 0
for _n, _w in (("bc_k", 4), ("bc_kr", 4), ("bc_ik", 1), ("bc_ikr", 1), ("bc_q", 4), ("bc_qr", 4), ("bc_iq", 4), ("bc_iqr", 4), ("bc_mq", 4), ("bc_gate", 24)):
    BCOLS[_n] = _o
    _o += _w
NBCOL = _o

CONST_SPECS = {
    "c_ident": ([128, 128], F32), "c_rope": ([128, 4], F32), "c_mask": ([128, 1024], F32),
    "c_pcur0": ([128, 4, 128], F32), "c_pcur": ([128, 4, 128], F32), "c_phal": ([128, 4, 128], F32), "c_phal7": ([16, 4, 128], F32),
}
WEIGHT_SPECS = {
    "w_in": ([D, DIN], F32), "b_in": ([DIN], F32), "w_rot": ([D, 1600], F32), "c_bcols": ([128, NBCOL], F32),
    "pool_w": ([4, 128, 128], F32), "c_psc": ([128, 4], F32), "w_mem_kv": ([D, 1024], F32), "w_br": ([3, 512, D], F32),
    "w_out": ([D, D], F32), "ln1_g": ([D], F32), "ln1_b": ([D], F32),
    "w1": ([16, D, 512], F32), "w3": ([16, D, 512], F32), "w2": ([16, 512, D], F32), "ln2_g": ([D], F32), "ln2_b": ([D], F32),
}
SCRATCH = {
    "qT_s": ([512, TL], BF16), "iqT_s": ([512, TL], BF16), "mqT_s": ([512, TL], BF16), "up_s": ([TL, 512], BF16), "iw_s": ([TL, 8], F32),
    "poolT_s": ([512, TL], BF16), "memT_s": ([512, TL], BF16), "dsaT_s": ([512, TL], BF16), "x1_s": ([TL, D], F32),
}
PAY = {"pay_k": ([512, TL], BF16), "pay_ik": ([64, TL], BF16), "pay_v": ([TL, 512], BF16), "pay_ut": ([256, 512], BF16)}
GATH = {"g_k": ([8, 128, 4, TL], BF16), "g_ik": ([8, 64, TL], BF16), "g_v": ([8, TL, 512], BF16), "g_ut": ([8, 256, 512], BF16)}


def setup_common(P, C, L, need_rope=True):
    nc, sy, d, dt = P.nc, P.sy, P.d, P.dt
    load_consts(P, C)
    C["bcols"] = P.sb("bcols", [128, NBCOL], F32)
    C["eps"] = P.sb("eps", [128, 1], F32)
    sy.dma("sp", lambda e: e.dma_start(out=C["bcols"][:], in_=d["c_bcols" + L]), reads=[dt["c_bcols" + L]], writes=[C["t_const"]])
    sy.op("dve", lambda e: e.memset(C["eps"][:], LN_EPS), writes=[C["t_const"]])
    for k, v in BCOLS.items():
        C[k] = v
    C["xT"] = P.sb("xT", [128, 8, TL], BF16)
    C["t_xT"] = Tile("xT")


def build_A(L=""):
    ext_in = {"x_own": ([TL, D], F32), "pos": ([TL], I32)}
    for k in ("c_ident", "c_rope"):
        ext_in[k] = CONST_SPECS[k]
    for k in ("w_in", "b_in", "w_rot", "c_bcols"):
        ext_in[k + L] = WEIGHT_SPECS[k]
    P = Prog(ext_in, dict(PAY), {})
    C = {}
    setup_common(P, C, L)
    alloc_rope(P, C)
    phase_X(P, C, "x_own")
    phase_A(P, C, L)
    P.finish()
    return P.nc


def build_B(L="", stop_after=None, outs=("x_out",)):
    ext_in = {"x_own": ([TL, D], F32), "pos": ([TL], I32), "mem": ([256, D], F32),
              "w_router": ([D, 16], F32), "router_bias": ([16], F32)}
    ext_in.update(CONST_SPECS)
    for k, v in WEIGHT_SPECS.items():
        ext_in[k + L] = v
    ext_in.update(GATH)
    allint = dict(SCRATCH)
    allint["x_out"] = ([TL, D], F32)
    ext_out = {k: allint.pop(k) for k in outs}
    P = Prog(ext_in, ext_out, allint)
    C = {}
    setup_common(P, C, L)
    mrope = P.mark()
    alloc_rope(P, C)
    phase_X(P, C, "x_own")
    steps = ["B1", "B2", "B3", "B4", "B5", "B6", "B7", "B8"]
    if stop_after is not None:
        steps = steps[:steps.index(stop_after) + 1]
    if "B1" in steps:
        phase_B1(P, C, L)
    P.release(mrope)
    if "B2" in steps:
        phase_B2(P, C, L)
    if "B3" in steps:
        phase_B3(P, C, L)
    if "B4" in steps:
        phase_B4(P, C, L)
    if "B5" in steps:
        C["x2T"] = P.sb("x2T", [128, 8, TL], BF16)
        C["t_x2T"] = Tile("x2T")
        C["logits"] = P.sb("logits", [128, 16, 16], F32)
        C["t_logits"] = Tile("logits")
        C["gate"] = P.sb("gate", [128, 16, 16], F32)
        C["t_gate"] = Tile("gate")
        phase_B5(P, C, L, "x_own")
    if "B6" in steps:
        phase_B6(P, C)
    if "B7" in steps:
        C["acc"] = P.sb("acc", [128, NTL, D], F32)
        C["t_acc"] = Tile("acc")
        phase_B7(P, C, L)
    if "B8" in steps:
        phase_B8(P, C, L, "x_out")
    P.finish()
    return P.nc


def rot_perm(n):
    idx = np.arange(n).reshape(-1, 64)
    return np.concatenate([idx[:, 32:], idx[:, :32]], axis=1).reshape(-1)


def host_consts():
    cs = {}
    cs["c_ident"] = np.eye(128, dtype=np.float32)
    p = np.arange(128)
    invf = (10000.0 ** (-(p % 32).astype(np.float64) / 32.0)).astype(np.float32)
    sgn = np.where((p % 64) < 32, -1.0, 1.0).astype(np.float32)
    TWO_PI = 6.2831
    cs["c_rope"] = np.stack([invf, sgn * TWO_PI, np.full(128, TWO_PI, np.float32), np.zeros(128, np.float32)], axis=1).astype(np.float32)
    wins = (2, 4, 8, 16)
    pcur = np.zeros((128, 4, 128), np.float32)
    phalo = np.zeros((16, 4, 128), np.float32)
    pcur0 = np.zeros((128, 4, 128), np.float32)
    for g, w in enumerate(wins):
        for t in range(128):
            for s_ in range(t - w + 1, t + 1):
                if s_ >= 0:
                    pcur[s_, g, t] += 1.0 / w
                else:
                    phalo[16 + s_, g, t] += 1.0 / w
            pcur[t, g, t] -= 1.0
            cnt = min(t + 1, w)
            for s_ in range(max(0, t - w + 1), t + 1):
                pcur0[s_, g, t] += 1.0 / cnt
            pcur0[t, g, t] -= 1.0
    per_core = []
    for c in range(NCORES):
        pc = {}
        t_ = np.arange(128)[:, None]
        k_ = np.arange(1024)[None, :]
        adm = (k_ // 64) <= (2 * c + t_ // 64)
        pc["c_mask"] = np.where(adm, 0.0, -1.0e30).astype(np.float32)
        pc["c_pcur0"] = pcur0 if c == 0 else pcur
        ph = np.zeros((128, 4, 128), np.float32)
        ph7 = np.zeros((16, 4, 128), np.float32)
        if c >= 1:
            ph[16 * (c - 1):16 * c] = phalo
        else:
            ph7[:] = phalo
        pc["c_phal"] = ph
        pc["c_phal7"] = ph7
        pc["c_pcur"] = pcur
        per_core.append(pc)
    return cs, per_core


def layer_weights(inp, l):
    w_in = np.ascontiguousarray(inp["w_in"][l])
    b_in = np.ascontiguousarray(inp["b_in"][l])
    cols = []
    for nm, n in (("q", 512), ("k", 512), ("iq", 512), ("ik", 64)):
        cols.append(OFF[nm] + rot_perm(n))
    cols = np.concatenate(cols)
    w_rot = np.ascontiguousarray(w_in[:, cols])
    b_rot = b_in[cols]
    bc = np.zeros((128, NBCOL), np.float32)

    def put(name, vec):
        n = (len(vec) + 127) // 128
        v = np.zeros(n * 128, np.float32)
        v[:len(vec)] = vec
        bc[:, BCOLS[name]:BCOLS[name] + n] = v.reshape(n, 128).T
    put("bc_k", b_in[OFF["k"]:OFF["k"] + 512])
    put("bc_kr", b_rot[ROFF["k"]:ROFF["k"] + 512])
    put("bc_ik", b_in[OFF["ik"]:OFF["ik"] + 64])
    put("bc_ikr", b_rot[ROFF["ik"]:ROFF["ik"] + 64])
    put("bc_q", b_in[OFF["q"]:OFF["q"] + 512])
    put("bc_qr", b_rot[ROFF["q"]:ROFF["q"] + 512])
    put("bc_iq", b_in[OFF["iq"]:OFF["iq"] + 512])
    put("bc_iqr", b_rot[ROFF["iq"]:ROFF["iq"] + 512])
    put("bc_mq", b_in[OFF["mq"]:OFF["mq"] + 512])
    put("bc_gate", b_in[OFF["gate"]:OFF["gate"] + 3072])
    W = {"w_in": w_in, "b_in": b_in, "w_rot": w_rot, "c_bcols": bc,
         "pool_w": np.ascontiguousarray(inp["pool_w"][l]),
         "c_psc": np.ascontiguousarray(inp["pool_scale"][l].reshape(4, 128).T),
         "w_mem_kv": np.ascontiguousarray(inp["w_mem_kv"][l]), "w_br": np.ascontiguousarray(inp["w_br"][l]),
         "w_out": np.ascontiguousarray(inp["w_out"][l]), "ln1_g": np.ascontiguousarray(inp["ln1_g"][l]),
         "ln1_b": np.ascontiguousarray(inp["ln1_b"][l]),
         "w1": np.ascontiguousarray(inp["w1"][l]), "w3": np.ascontiguousarray(inp["w3"][l]), "w2": np.ascontiguousarray(inp["w2"][l]),
         "ln2_g": np.ascontiguousarray(inp["ln2_g"][l]), "ln2_b": np.ascontiguousarray(inp["ln2_b"][l])}
    return W


def shard_tokens(x2d):
    F = x2d.shape[1]
    xb = x2d.reshape(128, 128, F)
    return [np.ascontiguousarray(xb[c::8].reshape(TL, F)) for c in range(NCORES)]


def unshard_tokens(parts):
    F = parts[0].shape[1]
    out = np.empty((128, 128, F), parts[0].dtype)
    for c in range(NCORES):
        out[c::8] = parts[c].reshape(NTL, 128, F)
    return out.reshape(S, F)


_CACHE = {}


def get_prog(kind):
    if kind not in _CACHE:
        _CACHE[kind] = build_A() if kind == "A" else build_B()
    return _CACHE[kind]


def kernel(**inp):
    inp = {k: np.asarray(v) for k, v in inp.items()}
    cs, per_core = host_consts()
    x_parts = shard_tokens(np.ascontiguousarray(inp["x"][0]))
    pos_parts = [p.reshape(TL) for p in shard_tokens(np.ascontiguousarray(inp["positions"][0]).reshape(S, 1))]
    mem = np.ascontiguousarray(inp["mem"][0])
    cores = list(range(NCORES))
    for l in range(2):
        W = layer_weights(inp, l)
        in_maps = []
        for c in cores:
            m = {"x_own": x_parts[c], "pos": pos_parts[c], "c_ident": cs["c_ident"], "c_rope": cs["c_rope"]}
            for k in ("w_in", "b_in", "w_rot", "c_bcols"):
                m[k] = W[k]
            in_maps.append(m)
        resA = run_bass_kernel_spmd(get_prog("A"), in_maps, core_ids=cores).results
        g = {"g_k": np.stack([np.asarray(r["pay_k"]).reshape(4, 128, TL).transpose(1, 0, 2) for r in resA]),
             "g_ik": np.stack([np.asarray(r["pay_ik"]) for r in resA]),
             "g_v": np.stack([np.asarray(r["pay_v"]) for r in resA]),
             "g_ut": np.stack([np.asarray(r["pay_ut"]) for r in resA])}
        g = {k: np.ascontiguousarray(v) for k, v in g.items()}
        in_maps = []
        for c in cores:
            m = {"x_own": x_parts[c], "pos": pos_parts[c], "mem": mem,
                 "w_router": np.ascontiguousarray(inp["w_router"]), "router_bias": np.ascontiguousarray(inp["router_bias"])}
            m.update(cs)
            m.update(per_core[c])
            m.update(W)
            m.update(g)
            in_maps.append(m)
        resB = run_bass_kernel_spmd(get_prog("B"), in_maps, core_ids=cores).results
        x_parts = [np.ascontiguousarray(np.asarray(r["x_out"], dtype=np.float32)) for r in resB]
    out = unshard_tokens(x_parts)
    return out.reshape(1, S, D).astype(np.float32)
```

```python
import numpy as np
import ml_dtypes
import concourse.bass as bass
import concourse.mybir as mybir
from concourse.bass_utils import run_bass_kernel_spmd

F32 = mybir.dt.float32
BF16 = mybir.dt.bfloat16
I32 = mybir.dt.int32
U8 = mybir.dt.uint8
AF = mybir.ActivationFunctionType
ALU = mybir.AluOpType
AX = mybir.AxisListType

NCORES = 8
S = 16384
D = 1024
TL = 2048
NTL = 16
DIN = 6216
ALPHA = 4.0 ** 0.25
LN_EPS = 1e-5
NIT = 24
BIS_W = 1024.0
OFF = dict(pool=0, q=512, k=1024, v=1536, iq=2048, ik=2560, iw=2624, mq=2632, gate=3144)
ROFF = dict(q=0, k=512, iq=1024, ik=1536)


class Sem:
    def __init__(self, handle, step):
        self.h = handle
        self.step = step
        self.count = 0


class Tile:
    __slots__ = ("name", "w", "r")

    def __init__(self, name=""):
        self.name = name
        self.w = {}
        self.r = {}


class Q:
    def __init__(self, name, sem):
        self.name = name
        self.sem = sem
        self.waited = {}
        self.ops = []
        self.pending = False


class Sync:
    def __init__(self, nc, n_dma_sems=48):
        self.nc = nc
        self.stack = []
        self.qs = {}
        for name in ("pe", "dve", "act", "pool", "sp"):
            cm = nc.semaphore("s_" + name)
            h = cm.__enter__()
            self.stack.append(cm)
            self.qs[name] = Q(name, Sem(h, 1))
        self.dma_sems = []
        for i in range(n_dma_sems):
            cm = nc.semaphore("d%d" % i)
            h = cm.__enter__()
            self.stack.append(cm)
            self.dma_sems.append(Sem(h, 16))
        self.dma_rr = 0

    def close(self):
        for cm in reversed(self.stack):
            cm.__exit__(None, None, None)

    def _needs(self, reads, writes):
        need = {}

        def add(s, v):
            if need.get(s, 0) < v:
                need[s] = v
        for t in reads:
            for s, v in t.w.items():
                add(s, v)
        for t in writes:
            for s, v in t.w.items():
                add(s, v)
            for s, v in t.r.items():
                add(s, v)
        return need

    def _emit_waits(self, q, need, skip_self=False):
        for s, v in need.items():
            if skip_self and s is q.sem:
                continue
            if q.waited.get(s, 0) >= v:
                continue
            q.waited[s] = v
            q.ops.append(("wait", s, v))

    def op(self, qname, fn, reads=(), writes=(), signal=True):
        q = self.qs[qname]
        need = self._needs(reads, writes)
        is_pe = qname == "pe"
        self._emit_waits(q, need, skip_self=is_pe)
        if not is_pe:
            signal = True
        if signal:
            q.sem.count += 1
            val = q.sem.count
            q.ops.append(("ins", fn, q.sem, 1))
            q.pending = False
        else:
            val = q.sem.count + 1
            q.ops.append(("ins", fn, None, 0))
            q.pending = True
        for t in reads:
            if t.r.get(q.sem, 0) < val:
                t.r[q.sem] = val
        for t in writes:
            t.w[q.sem] = val
            t.r = {}
        return val

    def dma(self, qname, fn, reads=(), writes=()):
        q = self.qs[qname]
        need = self._needs(reads, writes)
        sem = self.dma_sems[self.dma_rr % len(self.dma_sems)]
        self.dma_rr += 1
        if sem.count > 0:
            need[sem] = max(need.get(sem, 0), sem.count)
        self._emit_waits(q, need)
        sem.count += 16
        val = sem.count
        q.ops.append(("ins", fn, sem, 16))
        for t in reads:
            if t.r.get(sem, 0) < val:
                t.r[sem] = val
        for t in writes:
            t.w[sem] = val
            t.r = {}

    def coll(self, fn, reads=(), writes=()):
        q = self.qs["pool"]
        if not hasattr(self, "cc_sem"):
            cm = self.nc.semaphore("cc_sem")
            self.cc_sem = Sem(cm.__enter__(), 1)
            self.stack.append(cm)
        sem = self.cc_sem
        need = self._needs(reads, writes)
        if sem.count > 0:
            need[sem] = max(need.get(sem, 0), sem.count)
        self._emit_waits(q, need)
        sem.count += 1
        val = sem.count
        q.ops.append(("ins", fn, sem, 1))
        for t in reads:
            if t.r.get(sem, 0) < val:
                t.r[sem] = val
        for t in writes:
            t.w[sem] = val
            t.r = {}

    def barrier(self):
        allsems = [q.sem for q in self.qs.values()] + self.dma_sems + ([self.cc_sem] if hasattr(self, "cc_sem") else [])
        for q in self.qs.values():
            assert not q.pending
            need = {s: s.count for s in allsems if s.count > 0 and s is not q.sem}
            self._emit_waits(q, need)

    def flush(self):
        nc = self.nc
        for q in self.qs.values():
            assert not q.pending, "PE has un-signalled trailing instructions"
        with nc.Block() as block:
            def mk(q):
                def body(eng):
                    for o in q.ops:
                        if o[0] == "wait":
                            eng.wait_ge(o[1].h, o[2])
                        else:
                            ins = o[1](eng)
                            if o[2] is not None:
                                ins.then_inc(o[2].h, o[3])
                return body
            block.tensor(mk(self.qs["pe"]))
            block.vector(mk(self.qs["dve"]))
            block.scalar(mk(self.qs["act"]))
            block.gpsimd(mk(self.qs["pool"]))
            block.sync(mk(self.qs["sp"]))
        for q in self.qs.values():
            q.ops = []


class Prog:
    def __init__(self, ext_in, ext_out, internal):
        self.nc = bass.Bass("TRN2", target_bir_lowering=False)
        nc = self.nc
        self.d = {}
        self.dt = {}
        for name, (shape, dt) in ext_in.items():
            self.d[name] = nc.dram_tensor(name, list(shape), dt, kind="ExternalInput").ap()
            self.dt[name] = Tile(name)
        for name, (shape, dt) in ext_out.items():
            self.d[name] = nc.dram_tensor(name, list(shape), dt, kind="ExternalOutput").ap()
            self.dt[name] = Tile(name)
        for name, spec in internal.items():
            shape, dt = spec[0], spec[1]
            if len(spec) > 2:
                self.d[name] = nc.dram_tensor(name, list(shape), dt, kind="Internal", addr_space=spec[2]).ap()
            else:
                self.d[name] = nc.dram_tensor(name, list(shape), dt, kind="Internal").ap()
            self.dt[name] = Tile(name)
        self.sy = Sync(nc)
        self.ctxs = []
        self.out_names = list(ext_out.keys())

    def sb(self, name, shape, dt):
        self.uid = getattr(self, "uid", 0) + 1
        cm = self.nc.sbuf_tensor("sb%d_%s" % (self.uid, name), list(shape), dt)
        h = cm.__enter__()
        self.ctxs.append(cm)
        return h

    def ps(self, name, shape, dt):
        cm = self.nc.psum_tensor(name, list(shape), dt)
        h = cm.__enter__()
        self.ctxs.append(cm)
        return h

    def mark(self):
        return len(self.ctxs)

    def release(self, mark):
        self.sy.barrier()
        self.sy.flush()
        while len(self.ctxs) > mark:
            self.ctxs.pop().__exit__(None, None, None)

    def finish(self):
        sy = self.sy
        q = sy.qs["sp"]
        need = {}
        for n in self.out_names:
            t = self.dt[n]
            for s, v in t.w.items():
                need[s] = max(need.get(s, 0), v)
        sy._emit_waits(q, need)
        sy.barrier()
        sy.flush()
        while self.ctxs:
            self.ctxs.pop().__exit__(None, None, None)
        sy.close()


def bcast_rows(ap, nparts):
    return ap.partition_broadcast(nparts)


def load_consts(P, C):
    nc, sy, d, dt = P.nc, P.sy, P.d, P.dt
    C["ident"] = P.sb("ident", [128, 128], BF16)
    C["identf"] = P.sb("identf", [128, 128], F32)
    C["ones"] = P.sb("ones", [128, 128], BF16)
    C["t_const"] = Tile("const")
    tc_ = C["t_const"]
    sy.dma("pool", lambda e: e.dma_start(out=C["ident"][:], in_=d["c_ident"]), reads=[dt["c_ident"]], writes=[tc_])
    sy.dma("sp", lambda e: e.dma_start(out=C["identf"][:], in_=d["c_ident"]), reads=[dt["c_ident"]], writes=[tc_])
    sy.op("dve", lambda e: e.memset(C["ones"][:], 1.0), writes=[tc_])
    C["bank"] = [P.ps("bank%d" % i, [128, 512], F32) for i in range(7)]
    C["bankb"] = P.ps("bankb", [128, 1024], BF16)
    C["tb"] = [Tile("bank%d" % i) for i in range(7)]
    C["tbb"] = Tile("bankb")


def alloc_rope(P, C):
    C["cosT"] = P.sb("cosT", [128, TL], BF16)
    C["sinT"] = P.sb("sinT", [128, TL], BF16)
    C["t_rope"] = Tile("rope")


def phase_X(P, C, xname):
    nc, sy, d, dt = P.nc, P.sy, P.d, P.dt
    xT = C["xT"]
    t_xT = C["t_xT"]
    m = P.mark()
    xb = [P.sb("xb%d" % i, [128, 1024], BF16) for i in range(2)]
    t_xb = [Tile("xb%d" % i) for i in range(2)]
    for j in range(NTL):
        b = j % 2
        sy.dma("pool", lambda e, j=j, b=b: e.dma_start(out=xb[b][:], in_=d[xname][j * 128:(j + 1) * 128, :]),
               reads=[dt[xname]], writes=[t_xb[b]])
        for k in range(8):
            sy.op("pe", lambda e, k=k, b=b: e.transpose(C["bankb"][:, k * 128:(k + 1) * 128], xb[b][:, k * 128:(k + 1) * 128], C["ident"][:]),
                  reads=[t_xb[b], C["t_const"]], writes=[C["tbb"]], signal=(k == 7))
        sy.op("dve", lambda e, j=j: e.tensor_copy(xT[:, :, j * 128:(j + 1) * 128],
                                                   C["bankb"][:].rearrange("p (k t) -> p k t", k=8)),
              reads=[C["tbb"]], writes=[t_xT])
    cosT, sinT, t_rope = C["cosT"], C["sinT"], C["t_rope"]
    posi = P.sb("posi", [128, TL], I32)
    y = P.sb("ropy", [128, TL], F32)
    f = P.sb("ropf", [128, TL], F32)
    ki = P.sb("ropk", [128, TL], I32)
    kf = P.sb("ropkf", [128, TL], F32)
    t1 = P.sb("ropt1", [128, TL], F32)
    rc = P.sb("ropc", [128, 4], F32)
    tt = Tile("ropetmp")
    sy.dma("sp", lambda e: e.dma_start(out=posi[:], in_=bcast_rows(d["pos"], 128)), reads=[dt["pos"]], writes=[tt])
    sy.dma("sp", lambda e: e.dma_start(out=rc[:], in_=d["c_rope"]), reads=[dt["c_rope"]], writes=[tt])
    sy.op("dve", lambda e: e.tensor_copy(y[:], posi[:]), reads=[tt], writes=[tt])
    sy.op("dve", lambda e: e.tensor_scalar(y[:], y[:], rc[:, 0:1], float(1.0 / (2 * np.pi)), ALU.mult, ALU.mult), reads=[tt], writes=[tt])
    for which in (0, 1):
        if which == 1:
            sy.op("dve", lambda e: e.tensor_scalar(y[:], y[:], 0.25, None, ALU.add), reads=[tt], writes=[tt])
        sy.op("dve", lambda e: e.tensor_copy(ki[:], y[:]), reads=[tt], writes=[tt])
        sy.op("dve", lambda e: e.tensor_copy(kf[:], ki[:]), reads=[tt], writes=[tt])
        sy.op("dve", lambda e: e.tensor_tensor(f[:], y[:], kf[:], ALU.subtract), reads=[tt], writes=[tt])
        sy.op("dve", lambda e: e.tensor_scalar(t1[:], f[:], 0.5, None, ALU.is_gt), reads=[tt], writes=[tt])
        sy.op("dve", lambda e: e.tensor_tensor(f[:], f[:], t1[:], ALU.subtract), reads=[tt], writes=[tt])
        sy.op("dve", lambda e: e.tensor_scalar(t1[:], f[:], -0.5, None, ALU.is_lt), reads=[tt], writes=[tt])
        sy.op("dve", lambda e: e.tensor_tensor(f[:], f[:], t1[:], ALU.add), reads=[tt], writes=[tt])
        if which == 0:
            sy.op("act", lambda e: e.activation(sinT[:], f[:], AF.Sin, scale=rc[:, 1:2]), reads=[tt], writes=[t_rope])
        else:
            sy.op("act", lambda e: e.activation(cosT[:], f[:], AF.Sin, scale=rc[:, 2:3]), reads=[tt], writes=[t_rope])
    P.release(m)


def load_w(P, name, dst, cols, ncols, q="pool"):
    src = P.d[name][:, cols:cols + ncols].rearrange("(k p) c -> p k c", p=128)
    return lambda e: e.dma_start(out=dst, in_=src)


def fproj(P, C, wname, wcol, rname, rcol, ncols, bcol, brcol, out_name, W, Wr, tW, tmp):
    nc, sy, d, dt = P.nc, P.sy, P.d, P.dt
    xT, t_xT = C["xT"], C["t_xT"]
    bank, tb = C["bank"], C["tb"]
    bc = C["bcols"]
    rot = rname is not None
    sy.dma("pool", load_w(P, wname, W[:, :, 0:ncols], wcol, ncols), reads=[dt[wname]], writes=[tW[0]])
    if rot:
        sy.dma("pool", load_w(P, rname, Wr[:, :, 0:ncols], rcol, ncols), reads=[dt[rname]], writes=[tW[1]])
    nchunk = (ncols + 127) // 128
    outT, t_out, o1, o2, t_o = tmp
    for c in range(nchunk):
        m = min(128, ncols - c * 128)
        for tg in range(4):
            pa, pb = bank[(2 * tg) % 4], bank[(2 * tg + 1) % 4]
            ta, tbb_ = tb[(2 * tg) % 4], tb[(2 * tg + 1) % 4]
            for k in range(8):
                sy.op("pe", lambda e, c=c, m=m, k=k, tg=tg, pa=pa: e.matmul(pa[0:m, :], W[:, k, c * 128:c * 128 + m], xT[:, k, tg * 512:(tg + 1) * 512], start=(k == 0), stop=(k == 7)),
                      reads=[tW[0], t_xT], writes=[ta], signal=(k == 7))
            if rot:
                for k in range(8):
                    sy.op("pe", lambda e, c=c, m=m, k=k, tg=tg, pb=pb: e.matmul(pb[0:m, :], Wr[:, k, c * 128:c * 128 + m], xT[:, k, tg * 512:(tg + 1) * 512], start=(k == 0), stop=(k == 7)),
                          reads=[tW[1], t_xT], writes=[tbb_], signal=(k == 7))
                i = tg % 2
                sy.op("dve", lambda e, c=c, m=m, tg=tg, pa=pa, i=i: e.scalar_tensor_tensor(o1[i][0:m, :], pa[0:m, :], bc[0:m, bcol + c:bcol + c + 1], C["cosT"][0:m, tg * 512:(tg + 1) * 512], ALU.add, ALU.mult),
                      reads=[ta, C["t_rope"], C["t_const"]], writes=[t_o[i]])
                sy.op("dve", lambda e, c=c, m=m, tg=tg, pb=pb, i=i: e.scalar_tensor_tensor(o2[i][0:m, :], pb[0:m, :], bc[0:m, brcol + c:brcol + c + 1], C["sinT"][0:m, tg * 512:(tg + 1) * 512], ALU.add, ALU.mult),
                      reads=[tbb_, C["t_rope"], C["t_const"]], writes=[t_o[2 + i]])
                sy.op("pool", lambda e, c=c, m=m, tg=tg, i=i: e.tensor_tensor(outT[0:m, c, tg * 512:(tg + 1) * 512], o1[i][0:m, :], o2[i][0:m, :], ALU.add),
                      reads=[t_o[i], t_o[2 + i]], writes=[t_out])
            else:
                sy.op("act", lambda e, c=c, m=m, tg=tg, pa=pa: e.activation(outT[0:m, c, tg * 512:(tg + 1) * 512], pa[0:m, :], AF.Identity, bias=bc[0:m, bcol + c:bcol + c + 1]),
                      reads=[ta, C["t_const"]], writes=[t_out])
    if ncols >= 128:
        dst = d[out_name].rearrange("(c p) t -> p c t", p=128)
        sy.dma("sp", lambda e: e.dma_start(out=dst, in_=outT[:, 0:nchunk, :]), reads=[t_out], writes=[dt[out_name]])
    else:
        sy.dma("sp", lambda e: e.dma_start(out=d[out_name], in_=outT[0:ncols, 0, :]), reads=[t_out], writes=[dt[out_name]])


def tproj(P, C, wname, wcol, ncols, brow_name, W, tW, out_name, out_dt, osb, t_osb, rows=None, brow=None, t_brow=None):
    nc, sy, d, dt = P.nc, P.sy, P.d, P.dt
    xT, t_xT = C["xT"], C["t_xT"]
    bank, tb = C["bank"], C["tb"]
    sy.dma("pool", load_w(P, wname, W[:, :, 0:ncols], wcol, ncols), reads=[dt[wname]], writes=[tW])
    sy.dma("pool", lambda e: e.dma_start(out=brow[0:1, 0:ncols], in_=d[brow_name][wcol:wcol + ncols].rearrange("(o n) -> o n", o=1)),
           reads=[dt[brow_name]], writes=[t_brow])
    for j in range(NTL):
        pa, ta = bank[4 + j % 2], tb[4 + j % 2]
        if rows is None:
            t0, mrows = j * 128, 128
        else:
            t0, mrows = j * 128 + 128 - rows, rows
        for k in range(8):
            sy.op("pe", lambda e, k=k, pa=pa, t0=t0, mrows=mrows: e.matmul(pa[0:mrows, 0:ncols], xT[:, k, t0:t0 + mrows], W[:, k, 0:ncols], start=(k == 0), stop=False),
                  reads=[tW, t_xT], writes=[ta], signal=False)
        sy.op("pe", lambda e, pa=pa, mrows=mrows: e.matmul(pa[0:mrows, 0:ncols], C["ones"][0:1, 0:mrows], brow[0:1, 0:ncols], start=False, stop=True),
              reads=[t_brow, C["t_const"]], writes=[ta], signal=True)
        i = j % 2
        sy.op("act", lambda e, pa=pa, mrows=mrows, i=i: e.activation(osb[i][0:mrows, 0:ncols], pa[0:mrows, 0:ncols], AF.Copy),
              reads=[ta], writes=[t_osb[i]])
        if rows is None:
            sy.dma("sp", lambda e, j=j, i=i: e.dma_start(out=d[out_name][j * 128:(j + 1) * 128, :], in_=osb[i][:, 0:ncols]),
                   reads=[t_osb[i]], writes=[dt[out_name]])
        else:
            sy.dma("sp", lambda e, j=j, i=i: e.dma_start(out=d[out_name][j * rows:(j + 1) * rows, :], in_=osb[i][0:rows, 0:ncols]),
                   reads=[t_osb[i]], writes=[dt[out_name]])


def phase_A(P, C, L):
    nc, sy, d, dt = P.nc, P.sy, P.d, P.dt
    m = P.mark()
    W = P.sb("A_W", [128, 8, 512], BF16)
    Wr = P.sb("A_Wr", [128, 8, 512], BF16)
    tW = [Tile("A_W"), Tile("A_Wr")]
    outT = P.sb("A_outT", [128, 4, TL], BF16)
    o1 = [P.sb("A_o1%d" % i, [128, 512], F32) for i in range(2)]
    o2 = [P.sb("A_o2%d" % i, [128, 512], F32) for i in range(2)]
    tmp = (outT, Tile("A_outT"), o1, o2, [Tile("A_o%d" % i) for i in range(4)])
    fproj(P, C, "w_in" + L, OFF["k"], "w_rot" + L, ROFF["k"], 512, C["bc_k"], C["bc_kr"], "pay_k" + L, W, Wr, tW, tmp)
    fproj(P, C, "w_in" + L, OFF["ik"], "w_rot" + L, ROFF["ik"], 64, C["bc_ik"], C["bc_ikr"], "pay_ik" + L, W, Wr, tW, tmp)
    osb = [P.sb("A_osb%d" % i, [128, 512], BF16) for i in range(2)]
    t_osb = [Tile("A_osb%d" % i) for i in range(2)]
    brow = P.sb("A_brow", [1, 512], BF16)
    t_brow = Tile("A_brow")
    tproj(P, C, "w_in" + L, OFF["v"], 512, "b_in" + L, W, tW[0], "pay_v" + L, BF16, osb, t_osb, brow=brow, t_brow=t_brow)
    tproj(P, C, "w_in" + L, OFF["pool"], 512, "b_in" + L, W, tW[0], "pay_ut" + L, BF16, osb, t_osb, rows=16, brow=brow, t_brow=t_brow)
    P.release(m)


def phase_B1(P, C, L):
    nc, sy, d, dt = P.nc, P.sy, P.d, P.dt
    m = P.mark()
    W = P.sb("B1_W", [128, 8, 512], BF16)
    Wr = P.sb("B1_Wr", [128, 8, 512], BF16)
    tW = [Tile("B1_W"), Tile("B1_Wr")]
    outT = P.sb("B1_outT", [128, 4, TL], BF16)
    o1 = [P.sb("B1_o1%d" % i, [128, 512], F32) for i in range(2)]
    o2 = [P.sb("B1_o2%d" % i, [128, 512], F32) for i in range(2)]
    tmp = (outT, Tile("B1_outT"), o1, o2, [Tile("B1_o%d" % i) for i in range(4)])
    fproj(P, C, "w_in" + L, OFF["q"], "w_rot" + L, ROFF["q"], 512, C["bc_q"], C["bc_qr"], "qT_s", W, Wr, tW, tmp)
    fproj(P, C, "w_in" + L, OFF["iq"], "w_rot" + L, ROFF["iq"], 512, C["bc_iq"], C["bc_iqr"], "iqT_s", W, Wr, tW, tmp)
    fproj(P, C, "w_in" + L, OFF["mq"], None, 0, 512, C["bc_mq"], 0, "mqT_s", W, Wr, tW, tmp)
    osb = [P.sb("B1_osb%d" % i, [128, 512], BF16) for i in range(2)]
    osf = [P.sb("B1_osf%d" % i, [128, 8], F32) for i in range(2)]
    t_osb = [Tile("B1_osb%d" % i) for i in range(2)]
    t_osf = [Tile("B1_osf%d" % i) for i in range(2)]
    brow = P.sb("B1_brow", [1, 512], BF16)
    t_brow = Tile("B1_brow")
    tproj(P, C, "w_in" + L, OFF["pool"], 512, "b_in" + L, W, tW[0], "up_s", BF16, osb, t_osb, brow=brow, t_brow=t_brow)
    tproj(P, C, "w_in" + L, OFF["iw"], 8, "b_in" + L, W, tW[0], "iw_s", F32, osf, t_osf, brow=brow, t_brow=t_brow)
    P.release(m)


def phase_B2(P, C, L):
    nc, sy, d, dt = P.nc, P.sy, P.d, P.dt
    bank, tb = C["bank"], C["tb"]
    m = P.mark()
    pcur0 = P.sb("pcur0", [128, 4, 128], BF16)
    pcur = P.sb("pcur", [128, 4, 128], BF16)
    phal = P.sb("phal", [128, 4, 128], BF16)
    phal7 = P.sb("phal7", [16, 4, 128], BF16)
    poolw = P.sb("poolw", [128, 4, 128], BF16)
    psc = P.sb("psc", [128, 4], F32)
    tcn = Tile("B2c")
    sy.dma("pool", lambda e: e.dma_start(out=pcur0[:], in_=d["c_pcur0"]), reads=[dt["c_pcur0"]], writes=[tcn])
    sy.dma("pool", lambda e: e.dma_start(out=pcur[:], in_=d["c_pcur"]), reads=[dt["c_pcur"]], writes=[tcn])
    sy.dma("pool", lambda e: e.dma_start(out=phal[:], in_=d["c_phal"]), reads=[dt["c_phal"]], writes=[tcn])
    sy.dma("pool", lambda e: e.dma_start(out=phal7[:], in_=d["c_phal7"]), reads=[dt["c_phal7"]], writes=[tcn])
    sy.dma("pool", lambda e: e.dma_start(out=poolw[:], in_=d["pool_w" + L].rearrange("g c e -> c g e")), reads=[dt["pool_w" + L]], writes=[tcn])
    sy.dma("sp", lambda e: e.dma_start(out=psc[:], in_=d["c_psc" + L]), reads=[dt["c_psc" + L]], writes=[tcn])
    u = [P.sb("B2u%d" % i, [128, 512], BF16) for i in range(2)]
    uh = [P.sb("B2uh%d" % i, [128, 512], BF16) for i in range(2)]
    u7 = [P.sb("B2u7%d" % i, [16, 512], BF16) for i in range(2)]
    DT = [P.sb("B2DT%d" % i, [128, 4, 128], BF16) for i in range(2)]
    po = [P.sb("B2po%d" % i, [128, 4, 128], BF16) for i in range(2)]
    t_u = [Tile() for _ in range(2)]
    t_uh = [Tile() for _ in range(2)]
    t_u7 = [Tile() for _ in range(2)]
    t_DT = [Tile() for _ in range(2)]
    t_po = [Tile() for _ in range(2)]
    gut = d["g_ut" + L].rearrange("(r n) f -> r n f", r=8)
    t_gut = dt["g_ut" + L]
    for i in range(2):
        sy.op("dve", lambda e, i=i: e.memset(u7[i][:], 0.0), writes=[t_u7[i]])
    for j in range(NTL):
        i = j % 2
        sy.dma("sp", lambda e, j=j, i=i: e.dma_start(out=u[i][:], in_=d["up_s"][j * 128:(j + 1) * 128, :]), reads=[dt["up_s"]], writes=[t_u[i]])
        for r in range(8):
            sy.dma("sp", lambda e, j=j, i=i, r=r: e.dma_start(out=uh[i][16 * r:16 * r + 16, :], in_=gut[r, j * 16:(j + 1) * 16, :]),
                   reads=[t_gut], writes=[t_uh[i]])
        if j >= 1:
            sy.dma("sp", lambda e, j=j, i=i: e.dma_start(out=u7[i][:], in_=gut[7, (j - 1) * 16:j * 16, :]), reads=[t_gut], writes=[t_u7[i]])
        pc = pcur0 if j == 0 else pcur
        pD, tD = bank[0 + i], tb[0 + i]
        pM, tM = bank[2 + i], tb[2 + i]
        for g in range(4):
            o = pD[:, g * 128:(g + 1) * 128]
            sy.op("pe", lambda e, g=g, i=i, o=o, pc=pc: e.matmul(o, u[i][:, g * 128:(g + 1) * 128], pc[:, g, :], start=True, stop=False),
                  reads=[t_u[i], tcn], writes=[tD], signal=False)
            sy.op("pe", lambda e, g=g, i=i, o=o: e.matmul(o, uh[i][:, g * 128:(g + 1) * 128], phal[:, g, :], start=False, stop=False),
                  reads=[t_uh[i], tcn], writes=[tD], signal=False)
            sy.op("pe", lambda e, g=g, i=i, o=o: e.matmul(o, u7[i][0:16, g * 128:(g + 1) * 128], phal7[0:16, g, :], start=False, stop=True),
                  reads=[t_u7[i], tcn], writes=[tD], signal=(g == 3))
        sy.op("act", lambda e, i=i, pD=pD: e.activation(DT[i][:].rearrange("p g t -> p (g t)"), pD[:], AF.Copy), reads=[tD], writes=[t_DT[i]])
        for g in range(4):
            sy.op("pe", lambda e, g=g, i=i, pM=pM: e.matmul(pM[:, g * 128:(g + 1) * 128], poolw[:, g, :], DT[i][:, g, :], start=True, stop=True),
                  reads=[t_DT[i], tcn], writes=[tM], signal=(g == 3))
        for g in range(4):
            sy.op("dve", lambda e, g=g, i=i, pM=pM: e.tensor_scalar(po[i][:, g, :], pM[:, g * 128:(g + 1) * 128], psc[:, g:g + 1], None, ALU.mult),
                  reads=[tM, tcn], writes=[t_po[i]])
        dst = d["poolT_s"].rearrange("(g p) t -> p g t", p=128)[:, :, j * 128:(j + 1) * 128]
        sy.dma("sp", lambda e, i=i, dst=dst: e.dma_start(out=dst, in_=po[i][:]), reads=[t_po[i]], writes=[dt["poolT_s"]])
    P.release(m)


def phase_B3(P, C, L):
    nc, sy, d, dt = P.nc, P.sy, P.d, P.dt
    bank, tb = C["bank"], C["tb"]
    m = P.mark()
    memb = P.sb("memb", [128, 2, 1024], BF16)
    memT = P.sb("memT", [128, 8, 256], BF16)
    wkv = P.sb("wkv", [128, 8, 1024], BF16)
    mkT = P.sb("mkT", [128, 4, 256], BF16)
    mv = P.sb("mv", [128, 2, 512], BF16)
    t_memb, t_memT, t_wkv, t_mkT, t_mv = Tile(), Tile(), Tile(), Tile(), Tile()
    sy.dma("pool", lambda e: e.dma_start(out=memb[:], in_=d["mem"].rearrange("(a p) d -> p a d", p=128)), reads=[dt["mem"]], writes=[t_memb])
    sy.dma("pool", load_w(P, "w_mem_kv" + L, wkv[:], 0, 1024), reads=[dt["w_mem_kv" + L]], writes=[t_wkv])
    for a in range(2):
        for k in range(8):
            sy.op("pe", lambda e, a=a, k=k: e.transpose(C["bankb"][:, k * 128:(k + 1) * 128], memb[:, a, k * 128:(k + 1) * 128], C["ident"][:]),
                  reads=[t_memb, C["t_const"]], writes=[C["tbb"]], signal=(k == 7))
        sy.op("dve", lambda e, a=a: e.tensor_copy(memT[:, :, a * 128:(a + 1) * 128], C["bankb"][:].rearrange("p (k t) -> p k t", k=8)),
              reads=[C["tbb"]], writes=[t_memT])
    for h in range(4):
        pa, ta = bank[h % 2], tb[h % 2]
        for k in range(8):
            sy.op("pe", lambda e, h=h, k=k, pa=pa: e.matmul(pa[:, 0:256], wkv[:, k, h * 128:(h + 1) * 128], memT[:, k, :], start=(k == 0), stop=(k == 7)),
                  reads=[t_wkv, t_memT], writes=[ta], signal=(k == 7))
        sy.op("act", lambda e, h=h, pa=pa: e.activation(mkT[:, h, :], pa[:, 0:256], AF.Copy), reads=[ta], writes=[t_mkT])
    for a in range(2):
        pa, ta = bank[2 + a], tb[2 + a]
        for k in range(8):
            sy.op("pe", lambda e, a=a, k=k, pa=pa: e.matmul(pa[:], memT[:, k, a * 128:(a + 1) * 128], wkv[:, k, 512:1024], start=(k == 0), stop=(k == 7)),
                  reads=[t_wkv, t_memT], writes=[ta], signal=(k == 7))
        sy.op("act", lambda e, a=a, pa=pa: e.activation(mv[:, a, :], pa[:], AF.Copy), reads=[ta], writes=[t_mv])
    mq = [P.sb("B3mq%d" % i, [128, 4, 512], BF16) for i in range(2)]
    PT = [P.sb("B3PT%d" % i, [128, 512], BF16) for i in range(4)]
    rec = [P.sb("B3rec%d" % i, [128, 512], F32) for i in range(2)]
    mo = [P.sb("B3mo%d" % i, [128, 4, 512], BF16) for i in range(2)]
    t_mq = [Tile() for _ in range(2)]
    t_PT = [Tile() for _ in range(4)]
    t_rec = [Tile() for _ in range(2)]
    t_mo = [Tile() for _ in range(2)]
    scale = float(128.0 ** -0.5)
    for tg in range(4):
        i = tg % 2
        src = d["mqT_s"].rearrange("(h p) t -> p h t", p=128)[:, :, tg * 512:(tg + 1) * 512]
        sy.dma("sp", lambda e, i=i, src=src: e.dma_start(out=mq[i][:], in_=src), reads=[dt["mqT_s"]], writes=[t_mq[i]])
        for h in range(4):
            hh = h % 2
            for a in range(2):
                ps_, ts_ = bank[a], tb[a]
                sy.op("pe", lambda e, h=h, a=a, i=i, ps_=ps_: e.matmul(ps_[:], mkT[:, h, a * 128:(a + 1) * 128], mq[i][:, h, :], start=True, stop=True),
                      reads=[t_mkT, t_mq[i]], writes=[ts_], signal=True)
                sy.op("act", lambda e, a=a, hh=hh, ps_=ps_: e.activation(PT[2 * hh + a][:], ps_[:], AF.Exp, scale=scale), reads=[ts_], writes=[t_PT[2 * hh + a]])
            po_, to_ = bank[2 + hh], tb[2 + hh]
            pd_, td_ = bank[4 + hh], tb[4 + hh]
            for a in range(2):
                sy.op("pe", lambda e, h=h, a=a, hh=hh, po_=po_: e.matmul(po_[:], mv[:, a, h * 128:(h + 1) * 128], PT[2 * hh + a][:], start=(a == 0), stop=(a == 1)),
                      reads=[t_mv, t_PT[2 * hh + a]], writes=[to_], signal=(a == 1))
            for a in range(2):
                sy.op("pe", lambda e, a=a, hh=hh, pd_=pd_: e.matmul(pd_[:], C["ones"][:], PT[2 * hh + a][:], start=(a == 0), stop=(a == 1)),
                      reads=[C["t_const"], t_PT[2 * hh + a]], writes=[td_], signal=(a == 1))
            sy.op("dve", lambda e, hh=hh, pd_=pd_: e.reciprocal(rec[hh][:], pd_[:]), reads=[td_], writes=[t_rec[hh]])
            sy.op("dve", lambda e, h=h, hh=hh, i=i, po_=po_: e.tensor_tensor(mo[i][:, h, :], po_[:], rec[hh][:], ALU.mult),
                  reads=[to_, t_rec[hh]], writes=[t_mo[i]])
        dst = d["memT_s"].rearrange("(h p) t -> p h t", p=128)[:, :, tg * 512:(tg + 1) * 512]
        sy.dma("sp", lambda e, i=i, dst=dst: e.dma_start(out=dst, in_=mo[i][:]), reads=[t_mo[i]], writes=[dt["memT_s"]])
    P.release(m)


def phase_B4(P, C, L):
    nc, sy, d, dt = P.nc, P.sy, P.d, P.dt
    bank, tb = C["bank"], C["tb"]
    m = P.mark()
    ikT2 = P.sb("ikT2", [128, S], BF16)
    scores = P.sb("scores", [128, S], F32)
    junk = P.sb("junk", [128, S // 2], U8)
    cnt2 = P.sb("B4cnt2", [128, 1], F32)
    maskt = P.sb("maskt", [128, 1024], F32)
    t_ik, t_sc, t_junk, t_mt = Tile(), Tile(), Tile(), Tile()
    gik = d["g_ik" + L].rearrange("(r p) t -> r p t", r=8)
    for half in range(2):
        for r in range(8):
            dst = ikT2[64 * half:64 * half + 64, :].rearrange("p (j r t) -> p j r t", j=NTL, r=8)[:, :, r, :]
            src = gik[r].rearrange("p (j t) -> p j t", j=NTL)
            sy.dma("sp", lambda e, dst=dst, src=src: e.dma_start(out=dst, in_=src), reads=[dt["g_ik" + L]], writes=[t_ik])
    sy.dma("sp", lambda e: e.dma_start(out=maskt[:], in_=d["c_mask"]), reads=[dt["c_mask"]], writes=[t_mt])
    iq = [P.sb("B4iq%d" % i, [128, 4, 128], BF16) for i in range(2)]
    qq = [P.sb("B4q%d" % i, [128, 4, 128], BF16) for i in range(2)]
    iw = [P.sb("B4iw%d" % i, [128, 8], F32) for i in range(2)]
    diag = [P.sb("B4dg%d" % i, [128, 8, 128], BF16) for i in range(2)]
    t_iq = [Tile() for _ in range(2)]
    t_qq = [Tile() for _ in range(2)]
    t_iw = [Tile() for _ in range(2)]
    t_dg = [Tile() for _ in range(2)]
    R = [P.sb("B4R%d" % i, [128, 512], BF16) for i in range(3)]
    t_R = [Tile() for _ in range(3)]
    cnt = P.sb("B4cnt", [128, 1], F32)
    tmpc = P.sb("B4tmp", [128, 1], F32)
    mid = [P.sb("B4mid%d" % i, [128, 1], F32) for i in range(2)]
    thr = P.sb("B4thr", [128, 1], F32)
    nmid = [P.sb("B4nmid%d" % i, [128, 1], F32) for i in range(2)]
    sgn = P.sb("B4sgn", [128, 1], F32)
    junk2 = P.sb("junk2", [128, S // 2], U8)
    t_bis, t_cnt, t_sgn, t_junk2 = Tile(), Tile(), Tile(), Tile()
    Mk = [P.sb("B4M%d" % i, [128, 512], BF16) for i in range(2)]
    MT = [P.sb("B4MT%d" % i, [128, 512], BF16) for i in range(2)]
    t_Mk = [Tile() for _ in range(2)]
    t_MT = [Tile() for _ in range(2)]
    Kt = [P.sb("B4K%d" % i, [128, 4, 512], BF16) for i in range(2)]
    Vs = [P.sb("B4Vs%d" % i, [128, 4, 512], BF16) for i in range(2)]
    Vx = [P.sb("B4Vx%d" % i, [128, 4, 8, 128], BF16) for i in range(2)]
    t_K = [Tile() for _ in range(2)]
    t_Vs = [Tile() for _ in range(2)]
    t_Vx = [Tile() for _ in range(2)]
    E = [P.sb("B4E%d" % i, [128, 512], BF16) for i in range(3)]
    PTt = E
    t_E = [Tile() for _ in range(3)]
    t_PT = [Tile() for _ in range(3)]
    rec = P.sb("B4rec", [64, 8, 128], F32)
    oo = P.sb("B4oo", [64, 8, 128], BF16)
    dsab = [P.sb("B4dsab%d" % i, [128, 4, 128], BF16) for i in range(2)]
    t_rec, t_oo = Tile(), Tile()
    t_dsab = [Tile() for _ in range(2)]
    for i in range(2):
        sy.op("pool", lambda e, i=i: e.memset(Vx[i][:], 1.0), writes=[t_Vx[i]])
    gk = d["g_k" + L].rearrange("(r c p) t -> r p c t", r=8, c=4)
    gv = d["g_v" + L].rearrange("(r t) f -> r t f", r=8)
    qsrc = d["qT_s"].rearrange("(c p) t -> p c t", p=128)
    iqsrc = d["iqT_s"].rearrange("(c p) t -> p c t", p=128)
    acc = [bank[4], bank[5]]
    t_acc = [tb[4], tb[5]]
    kv_i = 0
    for j in range(NTL):
        b = j % 2
        ntile = 2 * (j + 1)
        Sj = 1024 * (j + 1)
        sy.dma("sp", lambda e, j=j, b=b: e.dma_start(out=iq[b][:], in_=iqsrc[:, :, j * 128:(j + 1) * 128]), reads=[dt["iqT_s"]], writes=[t_iq[b]])
        sy.dma("sp", lambda e, j=j, b=b: e.dma_start(out=qq[b][:], in_=qsrc[:, :, j * 128:(j + 1) * 128]), reads=[dt["qT_s"]], writes=[t_qq[b]])
        sy.dma("sp", lambda e, j=j, b=b: e.dma_start(out=iw[b][:], in_=d["iw_s"][j * 128:(j + 1) * 128, :]), reads=[dt["iw_s"]], writes=[t_iw[b]])
        for h in range(8):
            sy.op("pool", lambda e, h=h, b=b: e.tensor_scalar(diag[b][:, h, :], C["ident"][:], iw[b][:, h:h + 1], None, ALU.mult),
                  reads=[t_iw[b], C["t_const"]], writes=[t_dg[b]])
        for i in range(ntile):
            psc_, tsc_ = bank[2 + i % 2], tb[2 + i % 2]
            XB = (0, 1, 6)

            def emit_x(h, i=i, b=b):
                px, tx = bank[XB[h % 3]], tb[XB[h % 3]]
                hb = 64 * (h % 2)
                sy.op("pe", lambda e, h=h, i=i, b=b, px=px, hb=hb: e.matmul(px[:], iq[b][hb:hb + 64, h // 2, :], ikT2[hb:hb + 64, i * 512:(i + 1) * 512], start=True, stop=True),
                      reads=[t_iq[b], t_ik], writes=[tx], signal=True)
            emit_x(0)
            emit_x(1)
            for h in range(8):
                px, tx = bank[XB[h % 3]], tb[XB[h % 3]]
                ri = (i * 8 + h) % 3
                if h + 2 < 8:
                    emit_x(h + 2)
                if h % 2 == 0:
                    sy.op("act", lambda e, px=px, ri=ri: e.activation(R[ri][:], px[:], AF.Relu), reads=[tx], writes=[t_R[ri]])
                else:
                    sy.op("dve", lambda e, px=px, ri=ri: e.tensor_scalar(R[ri][:], px[:], 0.0, None, ALU.max), reads=[tx], writes=[t_R[ri]])
                sy.op("pe", lambda e, h=h, b=b, psc_=psc_, ri=ri: e.matmul(psc_[:], diag[b][:, h, :], R[ri][:], start=(h == 0), stop=(h == 7)),
                      reads=[t_dg[b], t_R[ri]], writes=[tsc_], signal=(h == 7))
            if i >= ntile - 2:
                mo = (i - (ntile - 2)) * 512
                sy.op("dve", lambda e, i=i, psc_=psc_, mo=mo: e.tensor_tensor(scores[:, i * 512:(i + 1) * 512], psc_[:], maskt[:, mo:mo + 512], ALU.add),
                      reads=[tsc_, t_mt], writes=[t_sc])
            else:
                sy.op("act", lambda e, i=i, psc_=psc_: e.activation(scores[:, i * 512:(i + 1) * 512], psc_[:], AF.Copy), reads=[tsc_], writes=[t_sc])
        Sa = max(int(Sj * 0.444) // 512 * 512, Sj - S // 2, 512)
        nact = Sj - Sa
        sy.op("dve", lambda e: e.memset(mid[0][:], 0.0), writes=[t_bis])
        sy.op("dve", lambda e: e.memset(nmid[0][:], 0.0), writes=[t_bis])
        for it in range(NIT):
            a, bb = it % 2, (it + 1) % 2
            step = BIS_W / (2.0 ** (it + 1))
            sy.op("dve", lambda e, a=a, Sa=Sa: e.tensor_scalar(junk[:, 0:Sa], scores[:, 0:Sa], mid[a][:, 0:1], None, ALU.is_ge, ALU.add, accum_out=cnt[:, 0:1]),
                  reads=[t_sc, t_bis], writes=[t_junk, t_cnt])
            sy.op("act", lambda e, a=a, Sa=Sa, Sj=Sj, nact=nact: e.activation(junk2[:, 0:nact], scores[:, Sa:Sj], AF.Sign, bias=nmid[a][:, 0:1], accum_out=sgn[:, 0:1]),
                  reads=[t_sc, t_bis], writes=[t_junk2, t_sgn])
            sy.op("dve", lambda e: e.scalar_tensor_tensor(cnt2[:], cnt[:], 2.0, sgn[:], ALU.mult, ALU.add), reads=[t_cnt, t_sgn], writes=[t_bis])
            sy.op("dve", lambda e, step=step, nact=nact: e.tensor_scalar(tmpc[:], cnt2[:], float(511.0 - nact), float(2.0 * step), ALU.is_ge, ALU.mult), reads=[t_bis], writes=[t_bis])
            sy.op("dve", lambda e, a=a, bb=bb, step=step: e.scalar_tensor_tensor(mid[bb][:], tmpc[:], float(-step), mid[a][:], ALU.add, ALU.add),
                  reads=[t_bis], writes=[t_bis])
            sy.op("dve", lambda e, bb=bb: e.tensor_scalar(nmid[bb][:], mid[bb][:], -1.0, None, ALU.mult), reads=[t_bis], writes=[t_bis])
        laststep = BIS_W / (2.0 ** NIT)
        sy.op("dve", lambda e: e.tensor_scalar(thr[:], mid[NIT % 2][:], float(-laststep), None, ALU.add), reads=[t_bis], writes=[t_bis])
        for i in range(ntile):
            kb = kv_i % 2
            kv_i += 1
            jj, r0 = i // 2, 4 * (i % 2)
            vsrc = gv[r0:r0 + 4, jj * 128:(jj + 1) * 128, :].rearrange("r t d -> t r d")
            for c in range(4):
                ksrc = gk[r0:r0 + 4, :, c, jj * 128:(jj + 1) * 128].rearrange("r p t -> p r t")
                sy.dma("sp", lambda e, kb=kb, ksrc=ksrc, c=c: e.dma_start(out=Kt[kb][:, c, :].rearrange("p (r t) -> p r t", r=4), in_=ksrc), reads=[dt["g_k" + L]], writes=[t_K[kb]])
            sy.dma("sp", lambda e, kb=kb, vsrc=vsrc: e.dma_start(out=Vs[kb][:], in_=vsrc), reads=[dt["g_v" + L]], writes=[t_Vs[kb]])
            sy.op("pool", lambda e, kb=kb: e.tensor_copy(Vx[kb][:, :, :, 0:64], Vs[kb][:].rearrange("p c (h d) -> p c h d", h=8)),
                  reads=[t_Vs[kb]], writes=[t_Vx[kb]])
            sy.op("dve", lambda e, i=i, kb=kb: e.tensor_scalar(Mk[kb][:], scores[:, i * 512:(i + 1) * 512], thr[:, 0:1], None, ALU.is_ge),
                  reads=[t_sc, t_bis], writes=[t_Mk[kb]])
            for c in range(4):
                sy.op("pe", lambda e, c=c, kb=kb: e.transpose(C["bankb"][:, c * 128:(c + 1) * 128], Mk[kb][:, c * 128:(c + 1) * 128], C["ident"][:]),
                      reads=[t_Mk[kb], C["t_const"]], writes=[C["tbb"]], signal=(c == 3))
            sy.op("act", lambda e, kb=kb: e.activation(MT[kb][:], C["bankb"][:, 0:512], AF.Copy), reads=[C["tbb"]], writes=[t_MT[kb]])
            def emit_st(h, kb=kb, b=b):
                pst, tst = bank[h % 4], tb[h % 4]
                hb = 64 * (h % 2)
                for c in range(4):
                    sy.op("pe", lambda e, h=h, c=c, kb=kb, b=b, pst=pst, hb=hb: e.matmul(pst[:, c * 128:(c + 1) * 128], Kt[kb][hb:hb + 64, h // 2, c * 128:(c + 1) * 128], qq[b][hb:hb + 64, h // 2, :], start=True, stop=True),
                          reads=[t_K[kb], t_qq[b]], writes=[tst], signal=(c == 3))
            emit_st(0)
            emit_st(1)
            for h in range(8):
                pst, tst = bank[h % 4], tb[h % 4]
                eb = h % 3
                if h + 2 < 8:
                    emit_st(h + 2)
                sy.op("act", lambda e, pst=pst, eb=eb: e.activation(E[eb][:], pst[:], AF.Exp, scale=0.125), reads=[tst], writes=[t_E[eb]])
                sy.op("dve", lambda e, eb=eb, kb=kb: e.tensor_tensor(PTt[eb][:], E[eb][:], MT[kb][:], ALU.mult), reads=[t_E[eb], t_MT[kb]], writes=[t_E[eb]])
                ab = h // 4
                for c in range(4):
                    first = (i == 0 and c == 0 and h % 4 == 0)
                    last = (i == ntile - 1 and c == 3)
                    sy.op("pe", lambda e, h=h, c=c, kb=kb, eb=eb, ab=ab, first=first, last=last: e.matmul(
                        acc[ab][:, (h % 4) * 128:(h % 4 + 1) * 128], Vx[kb][:, c, h, :], PTt[eb][:, c * 128:(c + 1) * 128],
                        start=first, stop=last, skip_group_check=True),
                        reads=[t_Vx[kb], t_E[eb]], writes=[t_acc[ab]], signal=(c == 3))
        for ab in range(2):
            sy.op("dve", lambda e, ab=ab: e.reciprocal(rec[0:64, 4 * ab:4 * ab + 4, :].rearrange("p h t -> p (h t)"), acc[ab][64:128, :]),
                  reads=[t_acc[ab]], writes=[t_rec])
            sy.op("dve", lambda e, ab=ab: e.tensor_tensor(oo[0:64, 4 * ab:4 * ab + 4, :].rearrange("p h t -> p (h t)"), acc[ab][0:64, :],
                                                          rec[0:64, 4 * ab:4 * ab + 4, :].rearrange("p h t -> p (h t)"), ALU.mult),
                  reads=[t_acc[ab], t_rec], writes=[t_oo])
        oov = oo[:].rearrange("p (a two) t -> p a two t", two=2)
        sy.op("dve", lambda e, b=b, oov=oov: e.tensor_copy(dsab[b][0:64, :, :], oov[0:64, :, 0, :]), reads=[t_oo], writes=[t_dsab[b]])
        sy.op("dve", lambda e, b=b, oov=oov: e.tensor_copy(dsab[b][64:128, :, :], oov[0:64, :, 1, :]), reads=[t_oo], writes=[t_dsab[b]])
        dst = d["dsaT_s"].rearrange("(a p) t -> p a t", p=128)[:, :, j * 128:(j + 1) * 128]
        sy.dma("sp", lambda e, b=b, dst=dst: e.dma_start(out=dst, in_=dsab[b][:]), reads=[t_dsab[b]], writes=[dt["dsaT_s"]])
    P.release(m)


def layer_norm_tile(P, C, y, t_y, stats, mv, sd, rstd, xn, gam, bet, out, t_tmp, t_ln, t_out):
    sy = P.sy
    for hlf in range(2):
        sy.op("dve", lambda e, hlf=hlf: e.bn_stats(stats[:, hlf * 6:(hlf + 1) * 6], y[:, hlf * 512:(hlf + 1) * 512]), reads=[t_y], writes=[t_tmp])
    sy.op("dve", lambda e: e.bn_aggr(mv[:], stats[:]), reads=[t_tmp], writes=[t_tmp])
    sy.op("act", lambda e: e.activation(sd[:], mv[:, 1:2], AF.Sqrt, bias=C["eps"][:, 0:1]), reads=[t_tmp, C["t_const"]], writes=[t_tmp])
    sy.op("dve", lambda e: e.reciprocal(rstd[:], sd[:]), reads=[t_tmp], writes=[t_tmp])
    sy.op("dve", lambda e: e.tensor_scalar(xn[:], y[:], mv[:, 0:1], rstd[:, 0:1], ALU.subtract, ALU.mult), reads=[t_y, t_tmp], writes=[t_tmp])
    sy.op("pool", lambda e: e.tensor_tensor(xn[:], xn[:], gam[:], ALU.mult), reads=[t_tmp, t_ln], writes=[t_tmp])
    sy.op("pool", lambda e: e.tensor_tensor(out[:], xn[:], bet[:], ALU.add), reads=[t_tmp, t_ln], writes=[t_out])


def phase_B5(P, C, L, xname):
    nc, sy, d, dt = P.nc, P.sy, P.d, P.dt
    bank, tb = C["bank"], C["tb"]
    xT, t_xT = C["xT"], C["t_xT"]
    x2T, t_x2T = C["x2T"], C["t_x2T"]
    m = P.mark()
    wbr = P.sb("wbr", [128, 3, 4, 1024], BF16)
    wout = P.sb("wout", [128, 8, 1024], BF16)
    wg = [P.sb("wg%d" % i, [128, 8, 1024], BF16) for i in range(1)] * 2
    wr = P.sb("wr", [128, 8, 16], F32)
    gam = P.sb("gam", [128, 1024], F32)
    bet = P.sb("bet", [128, 1024], F32)
    t_w, t_ln = Tile(), Tile()
    t_wg = [Tile()] * 2
    for n in range(3):
        sy.dma("pool", lambda e, n=n: e.dma_start(out=wbr[:, n, :, :], in_=d["w_br" + L][n].rearrange("(a c) d -> c a d", c=128)), reads=[dt["w_br" + L]], writes=[t_w])
    sy.dma("pool", load_w(P, "w_out" + L, wout[:], 0, 1024), reads=[dt["w_out" + L]], writes=[t_w])
    sy.dma("sp", lambda e: e.dma_start(out=wr[:], in_=d["w_router"].rearrange("(k p) e -> p k e", p=128)), reads=[dt["w_router"]], writes=[t_w])
    sy.dma("sp", lambda e: e.dma_start(out=gam[:], in_=bcast_rows(d["ln1_g" + L], 128)), reads=[dt["ln1_g" + L]], writes=[t_ln])
    sy.dma("sp", lambda e: e.dma_start(out=bet[:], in_=bcast_rows(d["ln1_b" + L], 128)), reads=[dt["ln1_b" + L]], writes=[t_ln])
    br = [P.sb("B5br%d" % i, [128, 4, 512], BF16) for i in range(2)]
    t_br = [Tile() for _ in range(2)]
    gt = [P.sb("B5g%d" % i, [128, 512], F32) for i in range(2)]
    t_gt = [Tile() for _ in range(2)]
    pg = [P.sb("B5pg%d" % i, [128, 512], F32) for i in range(2)]
    t_pg = [Tile() for _ in range(2)]
    mTf = P.sb("B5mTf", [128, 8, 512], F32)
    mTb = P.sb("B5mTb", [128, 8, 512], BF16)
    t_mTf, t_mTb = Tile(), Tile()
    xt = [P.sb("B5x%d" % i, [128, 1024], F32) for i in range(2)]
    t_xt = [Tile() for _ in range(2)]
    y = P.sb("B5y", [128, 1024], F32)
    xn = P.sb("B5xn", [128, 1024], F32)
    x1 = [P.sb("B5x1%d" % i, [128, 1024], F32) for i in range(2)]
    x1b = P.sb("B5x1b", [128, 1024], BF16)
    x1Tf = P.sb("B5x1Tf", [128, 8, 128], F32)
    stats = P.sb("B5st", [128, 12], F32)
    mv = P.sb("B5mv", [128, 2], F32)
    sd = P.sb("B5sd", [128, 1], F32)
    rstd = P.sb("B5rstd", [128, 1], F32)
    t_y, t_tmp, t_x1b, t_x1Tf = Tile(), Tile(), Tile(), Tile()
    t_x1 = [Tile() for _ in range(2)]
    names = ["poolT_s", "dsaT_s", "memT_s"]
    bcg = C["bc_gate"]
    li = 0
    for tg in range(4):
        for n in range(3):
            bi = li % 2
            li += 1
            src = d[names[n]].rearrange("(a p) t -> p a t", p=128)[:, :, tg * 512:(tg + 1) * 512]
            sy.dma("sp", lambda e, bi=bi, src=src: e.dma_start(out=br[bi][:], in_=src), reads=[dt[names[n]]], writes=[t_br[bi]])
            sy.dma("pool", load_w(P, "w_in" + L, wg[bi][:], OFF["gate"] + n * 1024, 1024), reads=[dt["w_in" + L]], writes=[t_wg[bi]])
            for dc in range(8):
                pp, tp = bank[dc % 2], tb[dc % 2]
                pq, tq = bank[2 + dc % 2], tb[2 + dc % 2]
                gi = dc % 2
                for a in range(4):
                    sy.op("pe", lambda e, n=n, a=a, dc=dc, bi=bi, pp=pp: e.matmul(pp[:], wbr[:, n, a, dc * 128:(dc + 1) * 128], br[bi][:, a, :], start=(a == 0), stop=(a == 3)),
                          reads=[t_w, t_br[bi]], writes=[tp], signal=(a == 3))
                for k in range(8):
                    sy.op("pe", lambda e, k=k, dc=dc, bi=bi, pq=pq, tg=tg: e.matmul(pq[:], wg[bi][:, k, dc * 128:(dc + 1) * 128], xT[:, k, tg * 512:(tg + 1) * 512], start=(k == 0), stop=(k == 7)),
                          reads=[t_wg[bi], t_xT], writes=[tq], signal=(k == 7))
                sy.op("act", lambda e, n=n, dc=dc, gi=gi, pq=pq: e.activation(gt[gi][:], pq[:], AF.Sigmoid, bias=C["bcols"][:, bcg + n * 8 + dc:bcg + n * 8 + dc + 1]),
                      reads=[tq, C["t_const"]], writes=[t_gt[gi]])
                if n == 0:
                    sy.op("dve", lambda e, dc=dc, gi=gi, pp=pp: e.tensor_tensor(mTf[:, dc, :], pp[:], gt[gi][:], ALU.mult), reads=[tp, t_gt[gi]], writes=[t_mTf])
                else:
                    sy.op("dve", lambda e, gi=gi, pp=pp: e.tensor_tensor(pg[gi][:], pp[:], gt[gi][:], ALU.mult), reads=[tp, t_gt[gi]], writes=[t_pg[gi]])
                    if n == 1:
                        sy.op("pool", lambda e, dc=dc, gi=gi: e.tensor_tensor(mTf[:, dc, :], mTf[:, dc, :], pg[gi][:], ALU.add), reads=[t_pg[gi], t_mTf], writes=[t_mTf])
                    else:
                        sy.op("pool", lambda e, dc=dc, gi=gi: e.tensor_tensor(mTb[:, dc, :], mTf[:, dc, :], pg[gi][:], ALU.add), reads=[t_pg[gi], t_mTf], writes=[t_mTb])
        for tl in range(4):
            j = tg * 4 + tl
            xi = j % 2
            sy.dma("sp", lambda e, j=j, xi=xi: e.dma_start(out=xt[xi][:], in_=d[xname][j * 128:(j + 1) * 128, :]), reads=[dt[xname]], writes=[t_xt[xi]])
            for hlf in range(2):
                pm, tm = bank[4 + hlf], tb[4 + hlf]
                for dc in range(8):
                    sy.op("pe", lambda e, dc=dc, hlf=hlf, tl=tl, pm=pm: e.matmul(pm[:], mTb[:, dc, tl * 128:(tl + 1) * 128], wout[:, dc, hlf * 512:(hlf + 1) * 512], start=(dc == 0), stop=(dc == 7)),
                          reads=[t_mTb, t_w], writes=[tm], signal=(dc == 7))
                sy.op("dve", lambda e, hlf=hlf, xi=xi, pm=pm: e.scalar_tensor_tensor(y[:, hlf * 512:(hlf + 1) * 512], xt[xi][:, hlf * 512:(hlf + 1) * 512], float(ALPHA), pm[:], ALU.mult, ALU.add),
                      reads=[tm, t_xt[xi]], writes=[t_y])
            layer_norm_tile(P, C, y, t_y, stats, mv, sd, rstd, xn, gam, bet, x1[xi], t_tmp, t_ln, t_x1[xi])
            sy.dma("sp", lambda e, j=j, xi=xi: e.dma_start(out=d["x1_s"][j * 128:(j + 1) * 128, :], in_=x1[xi][:]), reads=[t_x1[xi]], writes=[dt["x1_s"]])
            sy.op("act", lambda e, xi=xi: e.activation(x1b[:], x1[xi][:], AF.Copy), reads=[t_x1[xi]], writes=[t_x1b])
            for k in range(8):
                sy.op("pe", lambda e, k=k: e.transpose(C["bankb"][:, k * 128:(k + 1) * 128], x1b[:, k * 128:(k + 1) * 128], C["ident"][:]),
                      reads=[t_x1b, C["t_const"]], writes=[C["tbb"]], signal=(k == 7))
            sy.op("dve", lambda e, j=j: e.tensor_copy(x2T[:, :, j * 128:(j + 1) * 128], C["bankb"][:].rearrange("p (k t) -> p k t", k=8)),
                  reads=[C["tbb"]], writes=[t_x2T])
            for hlf in range(2):
                pf, tf = bank[hlf], tb[hlf]
                for kk in range(4):
                    k = hlf * 4 + kk
                    sy.op("pe", lambda e, k=k, kk=kk, xi=xi, pf=pf: e.transpose(pf[:, kk * 128:(kk + 1) * 128], x1[xi][:, k * 128:(k + 1) * 128], C["identf"][:]),
                          reads=[t_x1[xi], C["t_const"]], writes=[tf], signal=(kk == 3))
                sy.op("act", lambda e, hlf=hlf, pf=pf: e.activation(x1Tf[:, hlf * 4:(hlf + 1) * 4, :].rearrange("p k t -> p (k t)"), pf[:], AF.Copy), reads=[tf], writes=[t_x1Tf])
            pl, tl_ = bank[6], tb[6]
            for k in range(8):
                sy.op("pe", lambda e, k=k, pl=pl: e.matmul(pl[:, 0:16], x1Tf[:, k, :], wr[:, k, :], start=(k == 0), stop=(k == 7)),
                      reads=[t_x1Tf, t_w], writes=[tl_], signal=(k == 7))
            sy.op("act", lambda e, j=j, pl=pl: e.activation(C["logits"][:, j, :], pl[:, 0:16], AF.Copy), reads=[tl_], writes=[C["t_logits"]])
    P.release(m)


def phase_B6(P, C):
    nc, sy, d, dt = P.nc, P.sy, P.d, P.dt
    m = P.mark()
    t = Tile("router")
    lg = C["logits"]
    gate = C["gate"]
    BIG = 1.0e9
    rb = P.sb("R_rb", [128, 16], F32)
    sc = P.sb("R_sc", [128, 16, 16], F32)
    bi = P.sb("R_bi", [128, 16, 16], F32)
    b2 = P.sb("R_b2", [128, 16, 16], F32)
    eq = P.sb("R_eq", [128, 16, 16], F32)
    m1 = P.sb("R_m1", [128, 64], F32)
    m2 = P.sb("R_m2", [128, 64], F32)
    gs = P.sb("R_gs", [128, 64], F32)
    gmax = P.sb("R_gmax", [128, 16], F32)
    pen = P.sb("R_pen", [128, 64], F32)
    s1 = P.sb("R_s1", [128, 16], F32)
    s2 = P.sb("R_s2", [128, 16], F32)
    den = P.sb("R_den", [128, 16], F32)
    sy.dma("sp", lambda e: e.dma_start(out=rb[:], in_=bcast_rows(d["router_bias"], 128)), reads=[dt["router_bias"]], writes=[t])
    sy.op("act", lambda e: e.activation(sc[:], lg[:], AF.Sigmoid), reads=[C["t_logits"]], writes=[t])
    for j in range(NTL):
        sy.op("dve", lambda e, j=j: e.tensor_tensor(bi[:, j, :], sc[:, j, :], rb[:], ALU.add), reads=[t], writes=[t])
    v4 = lambda x: x[:].rearrange("p t (g e) -> p (t g) e", g=4)
    b64 = lambda x: x[:].unsqueeze(2).to_broadcast([128, 64, 4])
    b16 = lambda x: x[:].unsqueeze(2).to_broadcast([128, 16, 16])
    sy.op("dve", lambda e: e.tensor_reduce(m1[:], v4(bi), AX.X, ALU.max), reads=[t], writes=[t])
    sy.op("dve", lambda e: e.tensor_tensor(v4(eq), v4(bi), b64(m1), ALU.is_equal), reads=[t], writes=[t])
    sy.op("dve", lambda e: e.scalar_tensor_tensor(b2[:].rearrange("p t e -> p (t e)"), eq[:].rearrange("p t e -> p (t e)"), -BIG, bi[:].rearrange("p t e -> p (t e)"), ALU.mult, ALU.add), reads=[t], writes=[t])
    sy.op("dve", lambda e: e.tensor_reduce(m2[:], v4(b2), AX.X, ALU.max), reads=[t], writes=[t])
    sy.op("dve", lambda e: e.tensor_tensor(gs[:], m1[:], m2[:], ALU.add), reads=[t], writes=[t])
    sy.op("dve", lambda e: e.tensor_reduce(gmax[:], gs[:].rearrange("p (t g) -> p t g", g=4), AX.X, ALU.max), reads=[t], writes=[t])
    sy.op("dve", lambda e: e.tensor_tensor(pen[:].rearrange("p (t g) -> p t g", g=4), gs[:].rearrange("p (t g) -> p t g", g=4),
                                            gmax[:].unsqueeze(2).to_broadcast([128, 16, 4]), ALU.is_equal), reads=[t], writes=[t])
    sy.op("dve", lambda e: e.tensor_scalar(pen[:], pen[:], BIG, -BIG, ALU.mult, ALU.add), reads=[t], writes=[t])
    sy.op("dve", lambda e: e.tensor_tensor(v4(b2), v4(bi), b64(pen), ALU.add), reads=[t], writes=[t])
    sy.op("dve", lambda e: e.tensor_reduce(s1[:], b2[:], AX.X, ALU.max), reads=[t], writes=[t])
    sy.op("dve", lambda e: e.tensor_tensor(eq[:], b2[:], b16(s1), ALU.is_equal), reads=[t], writes=[t])
    sy.op("dve", lambda e: e.scalar_tensor_tensor(bi[:].rearrange("p t e -> p (t e)"), eq[:].rearrange("p t e -> p (t e)"), -BIG, b2[:].rearrange("p t e -> p (t e)"), ALU.mult, ALU.add), reads=[t], writes=[t])
    sy.op("dve", lambda e: e.tensor_reduce(s2[:], bi[:], AX.X, ALU.max), reads=[t], writes=[t])
    sy.op("dve", lambda e: e.tensor_tensor(eq[:], b2[:], b16(s2), ALU.is_ge), reads=[t], writes=[t])
    sy.op("dve", lambda e: e.tensor_tensor(b2[:], eq[:], sc[:], ALU.mult), reads=[t], writes=[t])
    sy.op("dve", lambda e: e.tensor_reduce(den[:], b2[:], AX.X, ALU.add), reads=[t], writes=[t])
    sy.op("dve", lambda e: e.reciprocal(den[:], den[:]), reads=[t], writes=[t])
    sy.op("dve", lambda e: e.tensor_tensor(gate[:], b2[:], b16(den), ALU.mult), reads=[t], writes=[C["t_gate"]])
    P.release(m)


def phase_B7(P, C, L):
    nc, sy, d, dt = P.nc, P.sy, P.d, P.dt
    bank, tb = C["bank"], C["tb"]
    x2T, t_x2T = C["x2T"], C["t_x2T"]
    acc, t_acc = C["acc"], C["t_acc"]
    gate = C["gate"]
    m = P.mark()
    w1 = [P.sb("w1_%d" % i, [128, 8, 512], BF16) for i in range(2)]
    w3 = [P.sb("w3_%d" % i, [128, 8, 512], BF16) for i in range(2)]
    w2 = [P.sb("w2_%d" % i, [128, 4, 1024], BF16) for i in range(2)]
    t_we = [Tile() for _ in range(2)]
    sl = [P.sb("B7s%d" % i, [128, 512], F32) for i in range(2)]
    t_sl = [Tile() for _ in range(2)]
    hT = [P.sb("B7h%d" % i, [128, 4, 512], BF16) for i in range(2)]
    t_hT = [Tile() for _ in range(2)]
    hi = 0
    for ex in range(16):
        wb = ex % 2
        sy.dma("pool", lambda e, ex=ex, wb=wb: e.dma_start(out=w1[wb][:], in_=d["w1" + L][ex].rearrange("(k p) f -> p k f", p=128)), reads=[dt["w1" + L]], writes=[t_we[wb]])
        sy.dma("pool", lambda e, ex=ex, wb=wb: e.dma_start(out=w3[wb][:], in_=d["w3" + L][ex].rearrange("(k p) f -> p k f", p=128)), reads=[dt["w3" + L]], writes=[t_we[wb]])
        sy.dma("pool", lambda e, ex=ex, wb=wb: e.dma_start(out=w2[wb][:], in_=d["w2" + L][ex].rearrange("(k p) f -> p k f", p=128)), reads=[dt["w2" + L]], writes=[t_we[wb]])
        for tg in range(4):
            hb = hi % 2
            hi += 1
            for fc in range(4):
                p1, t1 = bank[fc % 2], tb[fc % 2]
                p3, t3 = bank[2 + fc % 2], tb[2 + fc % 2]
                si = fc % 2
                for k in range(8):
                    sy.op("pe", lambda e, k=k, fc=fc, wb=wb, tg=tg, p1=p1: e.matmul(p1[:], w1[wb][:, k, fc * 128:(fc + 1) * 128], x2T[:, k, tg * 512:(tg + 1) * 512], start=(k == 0), stop=(k == 7)),
                          reads=[t_we[wb], t_x2T], writes=[t1], signal=(k == 7))
                for k in range(8):
                    sy.op("pe", lambda e, k=k, fc=fc, wb=wb, tg=tg, p3=p3: e.matmul(p3[:], w3[wb][:, k, fc * 128:(fc + 1) * 128], x2T[:, k, tg * 512:(tg + 1) * 512], start=(k == 0), stop=(k == 7)),
                          reads=[t_we[wb], t_x2T], writes=[t3], signal=(k == 7))
                sy.op("act", lambda e, si=si, p1=p1: e.activation(sl[si][:], p1[:], AF.Silu), reads=[t1], writes=[t_sl[si]])
                sy.op("dve", lambda e, fc=fc, hb=hb, si=si, p3=p3: e.tensor_tensor(hT[hb][:, fc, :], p3[:], sl[si][:], ALU.mult), reads=[t3, t_sl[si]], writes=[t_hT[hb]])
            for tl in range(4):
                j = tg * 4 + tl
                for hlf in range(2):
                    py, ty = bank[4 + hlf], tb[4 + hlf]
                    for fc in range(4):
                        sy.op("pe", lambda e, fc=fc, hb=hb, tl=tl, wb=wb, hlf=hlf, py=py: e.matmul(py[:], hT[hb][:, fc, tl * 128:(tl + 1) * 128], w2[wb][:, fc, hlf * 512:(hlf + 1) * 512], start=(fc == 0), stop=(fc == 3)),
                              reads=[t_hT[hb], t_we[wb]], writes=[ty], signal=(fc == 3))
                    if ex == 0:
                        sy.op("dve", lambda e, j=j, hlf=hlf, py=py, ex=ex: e.tensor_scalar(acc[:, j, hlf * 512:(hlf + 1) * 512], py[:], gate[:, j, ex:ex + 1], None, ALU.mult),
                              reads=[ty, C["t_gate"]], writes=[t_acc])
                    else:
                        sy.op("dve", lambda e, j=j, hlf=hlf, py=py, ex=ex: e.scalar_tensor_tensor(acc[:, j, hlf * 512:(hlf + 1) * 512], py[:], gate[:, j, ex:ex + 1], acc[:, j, hlf * 512:(hlf + 1) * 512], ALU.mult, ALU.add),
                              reads=[ty, C["t_gate"], t_acc], writes=[t_acc])
    P.release(m)


def phase_B8(P, C, L, out_name):
    nc, sy, d, dt = P.nc, P.sy, P.d, P.dt
    acc, t_acc = C["acc"], C["t_acc"]
    m = P.mark()
    gam = P.sb("gam2", [128, 1024], F32)
    bet = P.sb("bet2", [128, 1024], F32)
    t_ln = Tile()
    sy.dma("sp", lambda e: e.dma_start(out=gam[:], in_=bcast_rows(d["ln2_g" + L], 128)), reads=[dt["ln2_g" + L]], writes=[t_ln])
    sy.dma("sp", lambda e: e.dma_start(out=bet[:], in_=bcast_rows(d["ln2_b" + L], 128)), reads=[dt["ln2_b" + L]], writes=[t_ln])
    xt = [P.sb("B8x%d" % i, [128, 1024], F32) for i in range(2)]
    t_xt = [Tile() for _ in range(2)]
    y = P.sb("B8y", [128, 1024], F32)
    xn = P.sb("B8xn", [128, 1024], F32)
    xo = [P.sb("B8xo%d" % i, [128, 1024], F32) for i in range(2)]
    t_xo = [Tile() for _ in range(2)]
    stats = P.sb("B8st", [128, 12], F32)
    mv = P.sb("B8mv", [128, 2], F32)
    sd = P.sb("B8sd", [128, 1], F32)
    rstd = P.sb("B8rstd", [128, 1], F32)
    t_y, t_tmp = Tile(), Tile()
    for j in range(NTL):
        xi = j % 2
        sy.dma("sp", lambda e, j=j, xi=xi: e.dma_start(out=xt[xi][:], in_=d["x1_s"][j * 128:(j + 1) * 128, :]), reads=[dt["x1_s"]], writes=[t_xt[xi]])
        sy.op("dve", lambda e, j=j, xi=xi: e.scalar_tensor_tensor(y[:], xt[xi][:], float(ALPHA), acc[:, j, :], ALU.mult, ALU.add),
              reads=[t_xt[xi], t_acc], writes=[t_y])
        layer_norm_tile(P, C, y, t_y, stats, mv, sd, rstd, xn, gam, bet, xo[xi], t_tmp, t_ln, t_xo[xi])
        if out_name == "x_out":
            sy.dma("sp", lambda e, j=j, xi=xi: e.dma_start(out=d["x_out_%d" % j], in_=xo[xi][:]), reads=[t_xo[xi]], writes=[dt["x_out_%d" % j]])
        else:
            sy.dma("sp", lambda e, j=j, xi=xi: e.dma_start(out=d[out_name][j * 128:(j + 1) * 128, :], in_=xo[xi][:]), reads=[t_xo[xi]], writes=[dt[out_name]])
    P.release(m)


BCOLS = {}
_o = 0
for _n, _w in (("bc_k", 4), ("bc_kr", 4), ("bc_ik", 1), ("bc_ikr", 1), ("bc_q", 4), ("bc_qr", 4), ("bc_iq", 4), ("bc_iqr", 4), ("bc_mq", 4), ("bc_gate", 24)):
    BCOLS[_n] = _o
    _o += _w
NBCOL = _o

CONST_SPECS = {
    "c_ident": ([128, 128], F32), "c_rope": ([128, 4], F32), "c_mask": ([128, 1024], F32),
    "c_pcur0": ([128, 4, 128], F32), "c_pcur": ([128, 4, 128], F32), "c_phal": ([128, 4, 128], F32), "c_phal7": ([16, 4, 128], F32),
}
WEIGHT_SPECS = {
    "b_in": ([DIN], F32), "c_bcols": ([128, NBCOL], F32),
    "pool_w": ([4, 128, 128], F32), "c_psc": ([128, 4], F32),
    "ln1_g": ([D], F32), "ln1_b": ([D], F32), "ln2_g": ([D], F32), "ln2_b": ([D], F32),
}
BIGW = {
    "w_in": ([D, DIN], None), "w_rot": ([D, 1600], None), "w_mem_kv": ([D, 1024], None),
    "w_br": ([3 * 512, D], ("(n c) d -> n c d", dict(n=3))), "w_out": ([D, D], None),
    "w1": ([16 * D, 512], ("(e d) f -> e d f", dict(e=16))), "w3": ([16 * D, 512], ("(e d) f -> e d f", dict(e=16))),
    "w2": ([16 * 512, D], ("(e d) f -> e d f", dict(e=16))),
}
SCRATCH = {
    "qT_s": ([512, TL], BF16), "iqT_s": ([512, TL], BF16), "mqT_s": ([512, TL], BF16), "up_s": ([TL, 512], BF16), "iw_s": ([TL, 8], F32),
    "poolT_s": ([512, TL], BF16), "memT_s": ([512, TL], BF16), "dsaT_s": ([512, TL], BF16), "x1_s": ([TL, D], F32),
}
PAY = {"pay_k": ([512, TL], BF16), "pay_ik": ([64, TL], BF16), "pay_v": ([TL, 512], BF16), "pay_ut": ([256, 512], BF16)}
GATH = {"g_k": ([8 * 512, TL], BF16), "g_ik": ([8 * 64, TL], BF16), "g_v": ([8 * TL, 512], BF16), "g_ut": ([8 * 256, 512], BF16)}
LAYERS = ("_0", "_1")


def setup_common(P, C):
    nc, sy, d, dt = P.nc, P.sy, P.d, P.dt
    load_consts(P, C)
    C["eps"] = P.sb("eps", [128, 1], F32)
    sy.op("dve", lambda e: e.memset(C["eps"][:], LN_EPS), writes=[C["t_const"]])
    C["bcols_l"] = {}
    for L in LAYERS:
        C["bcols_l"][L] = P.sb("bcols" + L, [128, NBCOL], F32)
        sy.dma("sp", lambda e, L=L: e.dma_start(out=C["bcols_l"][L][:], in_=d["c_bcols" + L]), reads=[dt["c_bcols" + L]], writes=[C["t_const"]])
    for k, v in BCOLS.items():
        C[k] = v
    C["xT"] = P.sb("xT", [128, 8, TL], BF16)
    C["t_xT"] = Tile("xT")


def gather_phase(P, C, L):
    sy, d, dt = P.sy, P.d, P.dt
    grp = [list(range(NCORES))]
    sy.barrier()
    for nm in ("k", "ik", "v", "ut"):
        src, dst = "pay_" + nm + L, "g_" + nm + L
        sy.coll(lambda e, src=src, dst=dst: e.collective_compute("AllGather", ALU.bypass, replica_groups=grp, ins=[d[src].opt()], outs=[d[dst].opt()]),
                reads=[dt[src]], writes=[dt[dst]])
    sy.barrier()


def build_fused():
    ext_in = {"x_own": ([TL, D], F32), "pos": ([TL], I32),
              "w_router": ([D, 16], F32), "router_bias": ([16], F32)}
    ext_in.update(CONST_SPECS)
    internal = dict(SCRATCH)
    internal["x_mid"] = ([TL, D], F32)
    big = {"mem": ([256, D], None)}
    for L in LAYERS:
        for k, v in WEIGHT_SPECS.items():
            ext_in[k + L] = v
        for k, v in BIGW.items():
            big[k + L] = v
    for k, (shape, _) in big.items():
        ext_in[k + "_sh"] = ([shape[0] // NCORES, shape[1]], F32)
        internal[k + "_bn"] = ([shape[0] // NCORES, shape[1]], F32)
        internal[k + "_full"] = (list(shape), F32)
    for L in LAYERS:
        for k, v in PAY.items():
            internal[k + L] = v
        for k, v in GATH.items():
            internal[k + L] = v
    P = Prog(ext_in, {"x_out_%d" % j: ([128, D], F32) for j in range(NTL)}, internal)
    C = {}
    sy, d, dt = P.sy, P.d, P.dt
    grp = [list(range(NCORES))]
    for k, (shape, view) in big.items():
        sy.dma("sp", lambda e, k=k: e.dma_start(out=d[k + "_bn"], in_=d[k + "_sh"]), reads=[dt[k + "_sh"]], writes=[dt[k + "_bn"]])
    sy.barrier()
    for k, (shape, view) in big.items():
        sy.coll(lambda e, k=k: e.collective_compute("AllGather", ALU.bypass, replica_groups=grp, ins=[d[k + "_bn"].opt()], outs=[d[k + "_full"].opt()]),
                reads=[dt[k + "_bn"]], writes=[dt[k + "_full"]])
        d[k] = d[k + "_full"] if view is None else d[k + "_full"].rearrange(view[0], **view[1])
        dt[k] = dt[k + "_full"]
    sy.barrier()
    setup_common(P, C)
    for li, L in enumerate(LAYERS):
        xin = "x_own" if li == 0 else "x_mid"
        xout = "x_mid" if li == 0 else "x_out"
        C["bcols"] = C["bcols_l"][L]
        mrope = P.mark()
        alloc_rope(P, C)
        phase_X(P, C, xin)
        phase_A(P, C, L)
        gather_phase(P, C, L)
        phase_B1(P, C, L)
        P.release(mrope)
        phase_B2(P, C, L)
        phase_B3(P, C, L)
        phase_B4(P, C, L)
        m5 = P.mark()
        C["x2T"] = P.sb("x2T", [128, 8, TL], BF16)
        C["t_x2T"] = Tile("x2T")
        C["logits"] = P.sb("logits", [128, 16, 16], F32)
        C["t_logits"] = Tile("logits")
        C["gate"] = P.sb("gate", [128, 16, 16], F32)
        C["t_gate"] = Tile("gate")
        phase_B5(P, C, L, xin)
        phase_B6(P, C)
        C["acc"] = P.sb("acc", [128, NTL, D], F32)
        C["t_acc"] = Tile("acc")
        phase_B7(P, C, L)
        phase_B8(P, C, L, xout)
        P.release(m5)
    P.finish()
    return P.nc


def rot_perm(n):
    idx = np.arange(n).reshape(-1, 64)
    return np.concatenate([idx[:, 32:], idx[:, :32]], axis=1).reshape(-1)


def host_consts():
    cs = {}
    cs["c_ident"] = np.eye(128, dtype=np.float32)
    p = np.arange(128)
    invf = (10000.0 ** (-(p % 32).astype(np.float64) / 32.0)).astype(np.float32)
    sgn = np.where((p % 64) < 32, -1.0, 1.0).astype(np.float32)
    TWO_PI = 6.2831
    cs["c_rope"] = np.stack([invf, sgn * TWO_PI, np.full(128, TWO_PI, np.float32), np.zeros(128, np.float32)], axis=1).astype(np.float32)
    wins = (2, 4, 8, 16)
    pcur = np.zeros((128, 4, 128), np.float32)
    phalo = np.zeros((16, 4, 128), np.float32)
    pcur0 = np.zeros((128, 4, 128), np.float32)
    for g, w in enumerate(wins):
        for t in range(128):
            for s_ in range(t - w + 1, t + 1):
                if s_ >= 0:
                    pcur[s_, g, t] += 1.0 / w
                else:
                    phalo[16 + s_, g, t] += 1.0 / w
            pcur[t, g, t] -= 1.0
            cnt = min(t + 1, w)
            for s_ in range(max(0, t - w + 1), t + 1):
                pcur0[s_, g, t] += 1.0 / cnt
            pcur0[t, g, t] -= 1.0
    per_core = []
    for c in range(NCORES):
        pc = {}
        t_ = np.arange(128)[:, None]
        k_ = np.arange(1024)[None, :]
        adm = (k_ // 64) <= (2 * c + t_ // 64)
        pc["c_mask"] = np.where(adm, 0.0, -1.0e30).astype(np.float32)
        pc["c_pcur0"] = pcur0 if c == 0 else pcur
        ph = np.zeros((128, 4, 128), np.float32)
        ph7 = np.zeros((16, 4, 128), np.float32)
        if c >= 1:
            ph[16 * (c - 1):16 * c] = phalo
        else:
            ph7[:] = phalo
        pc["c_phal"] = ph
        pc["c_phal7"] = ph7
        pc["c_pcur"] = pcur
        per_core.append(pc)
    return cs, per_core


def layer_weights(inp, l):
    w_in = np.ascontiguousarray(inp["w_in"][l])
    b_in = np.ascontiguousarray(inp["b_in"][l])
    cols = []
    for nm, n in (("q", 512), ("k", 512), ("iq", 512), ("ik", 64)):
        cols.append(OFF[nm] + rot_perm(n))
    cols = np.concatenate(cols)
    w_rot = np.ascontiguousarray(w_in[:, cols])
    b_rot = b_in[cols]
    bc = np.zeros((128, NBCOL), np.float32)

    def put(name, vec):
        n = (len(vec) + 127) // 128
        v = np.zeros(n * 128, np.float32)
        v[:len(vec)] = vec
        bc[:, BCOLS[name]:BCOLS[name] + n] = v.reshape(n, 128).T
    put("bc_k", b_in[OFF["k"]:OFF["k"] + 512])
    put("bc_kr", b_rot[ROFF["k"]:ROFF["k"] + 512])
    put("bc_ik", b_in[OFF["ik"]:OFF["ik"] + 64])
    put("bc_ikr", b_rot[ROFF["ik"]:ROFF["ik"] + 64])
    put("bc_q", b_in[OFF["q"]:OFF["q"] + 512])
    put("bc_qr", b_rot[ROFF["q"]:ROFF["q"] + 512])
    put("bc_iq", b_in[OFF["iq"]:OFF["iq"] + 512])
    put("bc_iqr", b_rot[ROFF["iq"]:ROFF["iq"] + 512])
    put("bc_mq", b_in[OFF["mq"]:OFF["mq"] + 512])
    put("bc_gate", b_in[OFF["gate"]:OFF["gate"] + 3072])
    W = {"w_in": w_in, "b_in": b_in, "w_rot": w_rot, "c_bcols": bc,
         "pool_w": np.ascontiguousarray(inp["pool_w"][l]),
         "c_psc": np.ascontiguousarray(inp["pool_scale"][l].reshape(4, 128).T),
         "w_mem_kv": np.ascontiguousarray(inp["w_mem_kv"][l]), "w_br": np.ascontiguousarray(inp["w_br"][l]),
         "w_out": np.ascontiguousarray(inp["w_out"][l]), "ln1_g": np.ascontiguousarray(inp["ln1_g"][l]),
         "ln1_b": np.ascontiguousarray(inp["ln1_b"][l]),
         "w1": np.ascontiguousarray(inp["w1"][l]), "w3": np.ascontiguousarray(inp["w3"][l]), "w2": np.ascontiguousarray(inp["w2"][l]),
         "ln2_g": np.ascontiguousarray(inp["ln2_g"][l]), "ln2_b": np.ascontiguousarray(inp["ln2_b"][l])}
    return W


def shard_tokens(x2d):
    F = x2d.shape[1]
    xb = x2d.reshape(128, 128, F)
    return [np.ascontiguousarray(xb[c::8].reshape(TL, F)) for c in range(NCORES)]


def unshard_tokens(parts):
    F = parts[0].shape[1]
    out = np.empty((128, 128, F), parts[0].dtype)
    for c in range(NCORES):
        out[c::8] = parts[c].reshape(NTL, 128, F)
    return out.reshape(S, F)


_CACHE = {}


def get_prog():
    if "f" not in _CACHE:
        _CACHE["f"] = build_fused()
    return _CACHE["f"]


def kernel(**inp):
    inp = {k: np.asarray(v) for k, v in inp.items()}
    cs, per_core = host_consts()
    x_parts = shard_tokens(np.ascontiguousarray(inp["x"][0]))
    pos_parts = [p.reshape(TL) for p in shard_tokens(np.ascontiguousarray(inp["positions"][0]).reshape(S, 1))]
    mem = np.ascontiguousarray(inp["mem"][0])
    cores = list(range(NCORES))
    Ws = {}
    for li, L in enumerate(LAYERS):
        for k, v in layer_weights(inp, li).items():
            Ws[k + L] = v
    bigw = {"mem": mem}
    small = {}
    for k, v in Ws.items():
        if k[:-2] in BIGW:
            bigw[k] = np.ascontiguousarray(v).reshape(BIGW[k[:-2]][0])
        else:
            small[k] = v
    in_maps = []
    for c in cores:
        m = {"x_own": x_parts[c], "pos": pos_parts[c],
             "w_router": np.ascontiguousarray(inp["w_router"]), "router_bias": np.ascontiguousarray(inp["router_bias"])}
        m.update(cs)
        m.update(per_core[c])
        m.update(small)
        for k, v in bigw.items():
            n = v.shape[0] // NCORES
            m[k + "_sh"] = np.ascontiguousarray(v[c * n:(c + 1) * n])
        in_maps.append(m)
    res = run_bass_kernel_spmd(get_prog(), in_maps, core_ids=cores).results
    x_parts = [np.concatenate([np.asarray(r["x_out_%d" % j], dtype=np.float32) for j in range(NTL)], axis=0) for r in res]
    out = unshard_tokens(x_parts)
    return out.reshape(1, S, D).astype(np.float32)
```
